# Optimizing a Trainium2 kernel written in Bass

```python
import math
import jax, jax.numpy as jnp
from jax import lax
import numpy as np

D_MODEL = 4096
BATCH = 2
SEQ = 4096
DEPTH = 1

N_META = 16
SSM_D_INNER = 2 * D_MODEL
SSM_HEAD_DIM = 64
SSM_HEADS = SSM_D_INNER // SSM_HEAD_DIM
SSM_GROUPS = 8
SSM_STATE = 128
SSM_CONV = 4
SSM_CHUNK = 128
SSM_CONV_DIM = SSM_D_INNER + 2 * SSM_GROUPS * SSM_STATE
DT_MIN = 1e-3
DT_MAX = 1e-1
MLA_HEADS = 64
MLA_Q_RANK = D_MODEL // 4
MLA_KV_RANK = 512
MLA_NOPE = 128
MLA_ROPE = 64
MLA_V = 128
ROPE_THETA = 10000.0
ATTN_BLOCK = 128
N_BRANCH = 2
IN_WIDTHS = (SSM_D_INNER, SSM_CONV_DIM, SSM_HEADS, MLA_Q_RANK, MLA_KV_RANK, MLA_ROPE, N_BRANCH * D_MODEL)
IN_TOTAL = sum(IN_WIDTHS)
N_EXPERTS = 64
N_EXPERT_GROUPS = 8
TOPK_GROUPS = 4
TOP_K = 8
EXPERT_FF = 768
SHARED_FF = 768
ROUTED_SCALE = 2.5
MOE_BLOCK = 128
DN_ALPHA = (2.0 * DEPTH) ** 0.25
DN_BETA = (8.0 * DEPTH) ** -0.25
LN_EPS = 1e-5
RMS_EPS = 1e-6

kernel_name = 'hybrid_ssd_mla_moe_deepnorm_layer'


def layer_norm(x, g, b):
    xf = x.astype(jnp.float32)
    mu = jnp.mean(xf, axis=-1, keepdims=True)
    var = jnp.mean(jnp.square(xf - mu), axis=-1, keepdims=True)
    return ((xf - mu) * lax.rsqrt(var + LN_EPS) * g + b).astype(x.dtype)


def rms_norm(x, g):
    xf = x.astype(jnp.float32)
    return (xf * lax.rsqrt(jnp.mean(jnp.square(xf), axis=-1, keepdims=True) + RMS_EPS) * g).astype(x.dtype)


def rope_tables(pos):
    inv_freq = ROPE_THETA ** (-jnp.arange(0, MLA_ROPE, 2, dtype=jnp.float32) / MLA_ROPE)
    ang = pos.astype(jnp.float32)[..., None] * inv_freq
    return jnp.cos(ang), jnp.sin(ang)


def rotary(u, cos, sin):
    u1, u2 = jnp.split(u.astype(jnp.float32), 2, axis=-1)
    return jnp.concatenate([u1 * cos - u2 * sin, u2 * cos + u1 * sin], axis=-1)


def causal_depthwise_conv(u, w, b):
    k = w.shape[0]
    out = lax.conv_general_dilated(u, w[:, None, :].astype(u.dtype), window_strides=(1,), padding=[(k - 1, 0)],
                                   dimension_numbers=('NWC', 'WIO', 'NWC'), feature_group_count=u.shape[-1])
    return out + b


def ssd_mixer(z, xbc, dt_raw, conv_w, conv_b, dt_bias, a_log, d_skip, norm_g):
    bsz, seq_len, _ = z.shape
    q, g, hg, p, n = SSM_CHUNK, SSM_GROUPS, SSM_HEADS // SSM_GROUPS, SSM_HEAD_DIM, SSM_STATE
    xbc = jax.nn.silu(causal_depthwise_conv(xbc, conv_w, conv_b))
    xs, b_in, c_in = jnp.split(xbc, [SSM_D_INNER, SSM_D_INNER + g * n], axis=-1)
    dt = jax.nn.softplus((dt_raw + dt_bias).astype(jnp.float32))
    a = -jnp.exp(a_log.astype(jnp.float32)).reshape(g, hg)
    pad = (-N_META) % q
    pad_end = (-(pad + seq_len)) % q
    nc = (pad + seq_len + pad_end) // q

    def to_chunks(u, *tail):
        u = jnp.pad(u.astype(jnp.float32), ((0, 0), (pad, pad_end)) + ((0, 0),) * (u.ndim - 2))
        return u.reshape(bsz, nc, q, *tail)

    xs = to_chunks(xs, g, hg, p)
    b_c = to_chunks(b_in, g, n)
    c_c = to_chunks(c_in, g, n)
    dt_c = to_chunks(dt, g, hg)
    a_cs = jnp.cumsum(dt_c * a, axis=2)
    xdt = xs * dt_c[..., None]
    causal = jnp.tril(jnp.ones((q, q), dtype=bool))
    seg = a_cs[:, :, :, None] - a_cs[:, :, None, :]
    decay = jnp.exp(jnp.where(causal[:, :, None, None], seg, -jnp.inf))
    cb = jnp.einsum('bclgn,bcsgn->bclsg', c_c, b_c)
    y_diag = jnp.einsum('bclsgh,bcsghp->bclghp', decay * cb[..., None], xdt)
    decay_end = jnp.exp(a_cs[:, :, -1:] - a_cs)
    states = jnp.einsum('bcsgn,bcsghp->bcghpn', b_c, xdt * decay_end[..., None])
    chunk_decay = jnp.exp(a_cs[:, :, -1])

    def carry_state(h_prev, inp):
        st, dec = inp
        return h_prev * dec[..., None, None] + st, h_prev

    h0 = jnp.zeros((bsz, g, hg, p, n), jnp.float32)
    _, prev = lax.scan(carry_state, h0, (jnp.moveaxis(states, 1, 0), jnp.moveaxis(chunk_decay, 1, 0)))
    prev = jnp.moveaxis(prev, 0, 1)
    y_off = jnp.einsum('bclgn,bcghpn->bclghp', c_c, prev) * jnp.exp(a_cs)[..., None]
    y = y_diag + y_off + xs * d_skip.astype(jnp.float32).reshape(g, hg, 1)
    y = y.reshape(bsz, nc * q, SSM_D_INNER)[:, pad:pad + seq_len]
    yz = y * jax.nn.silu(z.astype(jnp.float32))
    yg = yz.reshape(bsz, seq_len, g, SSM_D_INNER // g)
    yg = yg * lax.rsqrt(jnp.mean(jnp.square(yg), axis=-1, keepdims=True) + RMS_EPS)
    return (yg.reshape(bsz, seq_len, SSM_D_INNER) * norm_g).astype(z.dtype)


def mla_mixer(q_a, kv_a, k_rope, cos, sin, q_a_norm_g, w_q_b, kv_a_norm_g, w_kv_b):
    bsz, seq_len, _ = q_a.shape
    hh = MLA_HEADS
    qf = (rms_norm(q_a, q_a_norm_g) @ w_q_b).reshape(bsz, seq_len, hh, MLA_NOPE + MLA_ROPE)
    q_nope, q_rope = jnp.split(qf, [MLA_NOPE], axis=-1)
    q_rope = rotary(q_rope, cos[:, :, None, :], sin[:, :, None, :]).astype(q_nope.dtype)
    kv = (rms_norm(kv_a, kv_a_norm_g) @ w_kv_b).reshape(bsz, seq_len, hh, MLA_NOPE + MLA_V)
    k_nope, v = jnp.split(kv, [MLA_NOPE], axis=-1)
    k_r = rotary(k_rope, cos, sin).astype(k_nope.dtype)
    scale = (MLA_NOPE + MLA_ROPE) ** -0.5
    nq = -(-seq_len // ATTN_BLOCK)
    lq = nq * ATTN_BLOCK

    def to_blocks(u):
        u = jnp.pad(u, ((0, 0), (0, lq - seq_len), (0, 0), (0, 0)))
        return jnp.moveaxis(u.reshape(bsz, nq, ATTN_BLOCK, *u.shape[2:]), 1, 0)

    key_pos = jnp.arange(seq_len)

    def attend_block(args):
        qn_b, qr_b, start = args
        s = (jnp.einsum('bqhd,bkhd->bhqk', qn_b, k_nope) + jnp.einsum('bqhr,bkr->bhqk', qr_b, k_r)).astype(jnp.float32) * scale
        qpos = start + jnp.arange(ATTN_BLOCK)
        s = jnp.where(key_pos[None, :] <= qpos[:, None], s, -jnp.inf)
        prob = jax.nn.softmax(s, axis=-1)
        return jnp.einsum('bhqk,bkhd->bqhd', prob.astype(v.dtype), v)

    starts = jnp.arange(nq, dtype=jnp.int32) * ATTN_BLOCK
    out = lax.map(attend_block, (to_blocks(q_nope), to_blocks(q_rope), starts))
    return jnp.moveaxis(out, 0, 1).reshape(bsz, lq, hh * MLA_V)[:, :seq_len]


def moe_ffn(h, w_router, router_bias, w_exp_gate, w_exp_up, w_exp_down, w_sh_gate, w_sh_up, w_sh_down):
    bsz, seq_len, d = h.shape
    t = bsz * seq_len
    xt = h.reshape(t, d)
    scores = jax.nn.sigmoid((xt @ w_router).astype(jnp.float32))
    choice = scores + router_bias.astype(jnp.float32)
    grp_score = lax.top_k(choice.reshape(t, N_EXPERT_GROUPS, -1), 2)[0].sum(-1)
    _, top_grp = lax.top_k(grp_score, TOPK_GROUPS)
    grp_mask = jnp.any(top_grp[..., None] == jnp.arange(N_EXPERT_GROUPS), axis=1)
    exp_mask = jnp.repeat(grp_mask, N_EXPERTS // N_EXPERT_GROUPS, axis=-1)
    _, top_e = lax.top_k(jnp.where(exp_mask, choice, -jnp.inf), TOP_K)
    wts = jnp.take_along_axis(scores, top_e, axis=-1)
    wts = wts / jnp.sum(wts, axis=-1, keepdims=True) * ROUTED_SCALE
    tk = t * TOP_K
    flat_e = top_e.reshape(tk)
    flat_tok = jnp.arange(tk, dtype=jnp.int32) // TOP_K
    flat_w = wts.reshape(tk)
    order = jnp.argsort(flat_e, stable=True)
    sorted_e = flat_e[order]
    counts = jnp.zeros((N_EXPERTS,), jnp.int32).at[flat_e].add(1)
    padded = (counts + MOE_BLOCK - 1) // MOE_BLOCK * MOE_BLOCK
    pad_end = jnp.cumsum(padded)
    pad_start = pad_end - padded
    start = jnp.cumsum(counts) - counts
    dest = pad_start[sorted_e] + jnp.arange(tk, dtype=jnp.int32) - start[sorted_e]
    n_blocks = -(-tk // MOE_BLOCK) + N_EXPERTS
    n_rows = n_blocks * MOE_BLOCK
    row_tok = jnp.full((n_rows,), t, jnp.int32).at[dest].set(flat_tok[order])
    row_w = jnp.zeros((n_rows,), jnp.float32).at[dest].set(flat_w[order])
    block_e = jnp.minimum(jnp.searchsorted(pad_end, jnp.arange(n_blocks, dtype=jnp.int32) * MOE_BLOCK, side='right'), N_EXPERTS - 1)
    x_pad = jnp.concatenate([xt, jnp.zeros((1, d), xt.dtype)], axis=0)

    def expert_block(acc, inp):
        tok, wt, e = inp
        xb = x_pad[tok]
        hb = jax.nn.silu(xb @ w_exp_gate[e]) * (xb @ w_exp_up[e])
        yb = (hb @ w_exp_down[e]).astype(jnp.float32) * wt[:, None]
        return acc.at[tok].add(yb), None

    acc0 = jnp.zeros((t + 1, d), jnp.float32)
    acc, _ = lax.scan(expert_block, acc0, (row_tok.reshape(n_blocks, MOE_BLOCK), row_w.reshape(n_blocks, MOE_BLOCK), block_e))
    shared = (jax.nn.silu(xt @ w_sh_gate) * (xt @ w_sh_up)) @ w_sh_down
    return (acc[:t] + shared.astype(jnp.float32)).astype(h.dtype).reshape(bsz, seq_len, d)


def setup_inputs(seed: int = 0) -> dict:
    key = jax.random.key(seed)
    ks = iter(jax.random.split(key, 48))

    def nrm(shape, scale):
        return jax.random.normal(next(ks), shape, jnp.float32) * scale

    def gain(shape):
        return 1.0 + nrm(shape, 0.01)

    x = nrm((BATCH, SEQ, D_MODEL), 1.0)
    positions = jnp.arange(SEQ, dtype=jnp.int32)[None, :] + jax.random.randint(next(ks), (BATCH, 1), 0, 1024, dtype=jnp.int32)
    meta_tokens = nrm((N_META, D_MODEL), 1.0)
    ln_in_g = gain((D_MODEL,))
    ln_in_b = nrm((D_MODEL,), 0.01)
    w_in = nrm((DEPTH, D_MODEL, IN_TOTAL), D_MODEL ** -0.5)
    b_gate = nrm((DEPTH, N_BRANCH * D_MODEL), 0.01)
    conv_w = nrm((DEPTH, SSM_CONV, SSM_CONV_DIM), SSM_CONV ** -0.5)
    conv_b = nrm((DEPTH, SSM_CONV_DIM), 0.01)
    u = jax.random.uniform(next(ks), (DEPTH, SSM_HEADS), jnp.float32)
    dt0 = jnp.exp(u * (math.log(DT_MAX) - math.log(DT_MIN)) + math.log(DT_MIN))
    dt_bias = dt0 + jnp.log(-jnp.expm1(-dt0))
    a_log = jnp.log(jax.random.uniform(next(ks), (DEPTH, SSM_HEADS), jnp.float32, 1.0, 16.0))
    d_skip = gain((DEPTH, SSM_HEADS))
    ssm_norm_g = gain((DEPTH, SSM_D_INNER))
    w_ssm_proj = nrm((DEPTH, SSM_D_INNER, D_MODEL), SSM_D_INNER ** -0.5 * DN_BETA)
    q_a_norm_g = gain((DEPTH, MLA_Q_RANK))
    w_q_b = nrm((DEPTH, MLA_Q_RANK, MLA_HEADS * (MLA_NOPE + MLA_ROPE)), MLA_Q_RANK ** -0.5)
    kv_a_norm_g = gain((DEPTH, MLA_KV_RANK))
    kv_scale = jnp.tile(jnp.concatenate([jnp.ones((MLA_NOPE,), jnp.float32), jnp.full((MLA_V,), DN_BETA, jnp.float32)]), MLA_HEADS)
    w_kv_b = nrm((DEPTH, MLA_KV_RANK, MLA_HEADS * (MLA_NOPE + MLA_V)), MLA_KV_RANK ** -0.5) * kv_scale
    w_attn_proj = nrm((DEPTH, MLA_HEADS * MLA_V, D_MODEL), (MLA_HEADS * MLA_V) ** -0.5 * DN_BETA)
    w_out = nrm((DEPTH, D_MODEL, D_MODEL), D_MODEL ** -0.5 * DN_BETA)
    ln1_g = gain((DEPTH, D_MODEL))
    ln1_b = nrm((DEPTH, D_MODEL), 0.01)
    w_router = nrm((DEPTH, D_MODEL, N_EXPERTS), D_MODEL ** -0.5)
    router_bias = nrm((DEPTH, N_EXPERTS), 0.01)
    w_exp_gate = nrm((DEPTH, N_EXPERTS, D_MODEL, EXPERT_FF), D_MODEL ** -0.5)
    w_exp_up = nrm((DEPTH, N_EXPERTS, D_MODEL, EXPERT_FF), D_MODEL ** -0.5 * DN_BETA)
    w_exp_down = nrm((DEPTH, N_EXPERTS, EXPERT_FF, D_MODEL), EXPERT_FF ** -0.5 * DN_BETA)
    w_sh_gate = nrm((DEPTH, D_MODEL, SHARED_FF), D_MODEL ** -0.5)
    w_sh_up = nrm((DEPTH, D_MODEL, SHARED_FF), D_MODEL ** -0.5 * DN_BETA)
    w_sh_down = nrm((DEPTH, SHARED_FF, D_MODEL), SHARED_FF ** -0.5 * DN_BETA)
    ln2_g = gain((DEPTH, D_MODEL))
    ln2_b = nrm((DEPTH, D_MODEL), 0.01)
    return {'x': x, 'positions': positions, 'meta_tokens': meta_tokens, 'ln_in_g': ln_in_g, 'ln_in_b': ln_in_b,
            'w_in': w_in, 'b_gate': b_gate, 'conv_w': conv_w, 'conv_b': conv_b, 'dt_bias': dt_bias, 'a_log': a_log,
            'd_skip': d_skip, 'ssm_norm_g': ssm_norm_g, 'w_ssm_proj': w_ssm_proj, 'q_a_norm_g': q_a_norm_g,
            'w_q_b': w_q_b, 'kv_a_norm_g': kv_a_norm_g, 'w_kv_b': w_kv_b, 'w_attn_proj': w_attn_proj, 'w_out': w_out,
            'ln1_g': ln1_g, 'ln1_b': ln1_b, 'w_router': w_router, 'router_bias': router_bias,
            'w_exp_gate': w_exp_gate, 'w_exp_up': w_exp_up, 'w_exp_down': w_exp_down,
            'w_sh_gate': w_sh_gate, 'w_sh_up': w_sh_up, 'w_sh_down': w_sh_down, 'ln2_g': ln2_g, 'ln2_b': ln2_b}


def reference(x, positions, meta_tokens, ln_in_g, ln_in_b, w_in, b_gate, conv_w, conv_b, dt_bias, a_log, d_skip,
              ssm_norm_g, w_ssm_proj, q_a_norm_g, w_q_b, kv_a_norm_g, w_kv_b, w_attn_proj, w_out, ln1_g, ln1_b,
              w_router, router_bias, w_exp_gate, w_exp_up, w_exp_down, w_sh_gate, w_sh_up, w_sh_down, ln2_g, ln2_b):
    bsz = x.shape[0]
    meta = jnp.broadcast_to(meta_tokens[None].astype(x.dtype), (bsz, N_META, D_MODEL))
    h = layer_norm(jnp.concatenate([meta, x], axis=1), ln_in_g, ln_in_b)
    pos = jnp.concatenate([jnp.broadcast_to(jnp.arange(N_META, dtype=jnp.int32), (bsz, N_META)),
                           positions.astype(jnp.int32) + N_META], axis=1)
    cos, sin = rope_tables(pos)
    offs = np.cumsum(IN_WIDTHS)[:-1].tolist()
    for l in range(DEPTH):
        proj = h @ w_in[l]
        z, xbc, dt_raw, q_a, kv_a, k_rope, gate_pre = jnp.split(proj, offs, axis=-1)
        gates = jax.nn.sigmoid((gate_pre + b_gate[l]).astype(jnp.float32)).astype(h.dtype)
        g_ssm, g_attn = jnp.split(gates, N_BRANCH, axis=-1)
        y_ssm = ssd_mixer(z, xbc, dt_raw, conv_w[l], conv_b[l], dt_bias[l], a_log[l], d_skip[l], ssm_norm_g[l]) @ w_ssm_proj[l]
        y_attn = mla_mixer(q_a, kv_a, k_rope, cos, sin, q_a_norm_g[l], w_q_b[l], kv_a_norm_g[l], w_kv_b[l]) @ w_attn_proj[l]
        mixed = (g_ssm * y_ssm + g_attn * y_attn) @ w_out[l]
        h = layer_norm(DN_ALPHA * h + mixed, ln1_g[l], ln1_b[l])
        ffn = moe_ffn(h, w_router[l], router_bias[l], w_exp_gate[l], w_exp_up[l], w_exp_down[l],
                      w_sh_gate[l], w_sh_up[l], w_sh_down[l])
        h = layer_norm(DN_ALPHA * h + ffn, ln2_g[l], ln2_b[l])
    return h[:, N_META:, :]
```

```python
import contextlib
import math
import numpy as np
import concourse.bass as bass
import concourse.mybir as mybir
from concourse.bass_utils import run_bass_kernel_spmd

F32 = mybir.dt.float32
BF16 = mybir.dt.bfloat16
I32 = mybir.dt.int32
AF = mybir.ActivationFunctionType
ALU = mybir.AluOpType

ENGS = ["pe", "act", "dve", "pool", "sp"]
NDMASEM = 12
class _Rec:
    def __getattr__(self, name):
        def f(*a, **k):
            self.call = (name, a, k)
        return f


class Prog:
    def __init__(self, nc):
        self.nc = nc
        self.ops = {e: [] for e in ENGS}
        self.lastw = {}
        self.readers = {}
        self.waited = {e: {} for e in ENGS}
        self.dcnt = {q: [0] * NDMASEM for q in ("sp", "pool", "act")}
        self.dnext = {q: 0 for q in ("sp", "pool", "act")}
        self.stack = contextlib.ExitStack()
        self.out_toks = []

    def sb(self, name, shape, dt):
        return self.stack.enter_context(self.nc.sbuf_tensor(name, list(shape), dt))

    def ps(self, name, shape, dt):
        return self.stack.enter_context(self.nc.psum_tensor(name, list(shape), dt))

    def _deps(self, eng, reads, writes):
        deps = []
        for k in reads:
            if k in self.lastw:
                deps.append((self.lastw[k], "raw"))
        for k in writes:
            if k in self.lastw:
                deps.append((self.lastw[k], "waw"))
            r = self.readers.get(k)
            if r:
                for e, i in r["eng"].items():
                    deps.append((("eng", e, i), "war"))
                for t in r["dma"]:
                    deps.append((t, "war"))
        waits = []
        for d, kind in deps:
            if d[0] == "eng":
                _, E, i = d
                if E == eng:
                    if E in ("pe", "sp"):
                        continue
                    if kind != "raw":
                        continue
                if self.waited[eng].get(E, -1) >= i:
                    continue
                self.waited[eng][E] = i
                self.ops[E][i]["sig"] = True
                waits.append(d)
            else:
                _, q, slot, v = d
                key = (q, slot)
                if self.waited[eng].get(key, 0) >= v:
                    continue
                self.waited[eng][key] = v
                waits.append(d)
        return waits

    def _record(self, tok, reads, writes):
        for k in reads:
            r = self.readers.setdefault(k, {"eng": {}, "dma": []})
            if tok[0] == "eng":
                r["eng"][tok[1]] = tok[2]
            else:
                r["dma"].append(tok)
        for k in writes:
            self.lastw[k] = tok
            self.readers[k] = {"eng": {}, "dma": []}

    def op(self, eng, fn, reads=(), writes=()):
        rec = _Rec()
        fn(rec)
        name, a, k = rec.call
        fn = lambda h, name=name, a=a, k=k: getattr(h, name)(*a, **k)
        waits = self._deps(eng, reads, writes)
        idx = len(self.ops[eng])
        self.ops[eng].append({"fn": fn, "waits": waits, "sig": False, "dma": None})
        self._record(("eng", eng, idx), reads, writes)

    def dma(self, q, out, in_, reads=(), writes=(), is_out=False, **kw):
        waits = self._deps(q, reads, writes)
        slot = self.dnext[q]
        self.dnext[q] = (slot + 1) % NDMASEM
        prev = self.dcnt[q][slot]
        if prev > 0 and self.waited[q].get((q, slot), 0) < 16 * prev:
            self.waited[q][(q, slot)] = 16 * prev
            waits.append(("dma", q, slot, 16 * prev))
        self.dcnt[q][slot] = prev + 1
        tok = ("dma", q, slot, 16 * (prev + 1))
        self.ops[q].append({"fn": lambda e, o=out, i=in_, kw=kw: e.dma_start(out=o, in_=i, **kw),
                            "waits": waits, "sig": False, "dma": (q, slot)})
        self._record(tok, reads, writes)
        if is_out:
            self.out_toks.append(tok)
        return tok

    def finish(self):
        waits = []
        for t in self.out_toks:
            _, q, slot, v = t
            if self.waited["sp"].get((q, slot), 0) >= v:
                continue
            self.waited["sp"][(q, slot)] = v
            waits.append(t)
        self.ops["sp"].append({"fn": None, "waits": waits, "sig": False, "dma": None})

    def emit(self):
        nc = self.nc
        st = self.stack
        sems = {e: st.enter_context(nc.semaphore("s_" + e)) for e in ENGS}
        dsem = {q: [st.enter_context(nc.semaphore("d_%s%d" % (q, i))) for i in range(NDMASEM)]
                for q in ("sp", "pool", "act")}
        for e in ENGS:
            c = 0
            for o in self.ops[e]:
                if o["sig"]:
                    c += 1
                    o["sv"] = c
        ops = self.ops

        def run(e, h):
            for o in ops[e]:
                for w in o["waits"]:
                    if w[0] == "eng":
                        h.wait_ge(sems[w[1]], ops[w[1]][w[2]]["sv"])
                    else:
                        h.wait_ge(dsem[w[1]][w[2]], w[3])
                if o["fn"] is None:
                    continue
                ins = o["fn"](h)
                if o["dma"] is not None:
                    ins.then_inc(dsem[o["dma"][0]][o["dma"][1]], 16)
                elif o["sig"]:
                    ins.then_inc(sems[e], 1)

        block = st.enter_context(nc.Block())

        @block.tensor
        def _(h):
            run("pe", h)

        @block.scalar
        def _(h):
            run("act", h)

        @block.vector
        def _(h):
            run("dve", h)

        @block.gpsimd
        def _(h):
            run("pool", h)

        @block.sync
        def _(h):
            run("sp", h)

        st.close()


D = 4096
NT = 33
L = NT * 128
JPB = 2
NQ = 4096 // JPB
QT0 = NT - NQ // 128
NCORES = 2 * JPB
EPS_LN = 1e-5
EPS_RMS = 1e-6
DN_ALPHA = 2.0 ** 0.25
OZ, OX, OB, OC, ODT, OQA, OKVA, OKR, OG = 0, 8192, 16384, 17408, 18432, 18560, 19584, 20096, 20160
ARENA = 50 * 1024


class Arena:
    def __init__(self, t, n):
        self.t, self.n, self.off = t, n, 0

    def f32(self, n):
        v = self.t[:, self.off:self.off + n]
        self.off += n
        assert self.off <= self.n, "arena overflow %d" % self.off
        return v

    def bf16(self, n):
        nn = (n + 1) // 2
        v = self.t[:, self.off:self.off + nn].bitcast(BF16)
        self.off += nn
        assert self.off <= self.n, "arena overflow %d" % self.off
        return v

    def i32(self, n):
        v = self.t[:, self.off:self.off + n].bitcast(I32)
        self.off += n
        assert self.off <= self.n, "arena overflow %d" % self.off
        return v


class Ctx:
    pass


def setup(nc, names):
    P = Prog(nc)
    C = Ctx()
    C.P, C.nc = P, nc
    shapes = dict(
        xin=([L, D], F32), validtm=([128, NT], F32), validrow=([1, L], F32), posraw=([1, L], I32), posoff=([1, L], I32),
        ln_in_g=([D], F32), ln_in_b=([D], F32), w_in=([D, 28352], F32), b_gate=([8192], F32),
        conv_w=([4, 10240], F32), conv_b=([10240], F32), dt_bias=([128], F32), a_log=([128], F32), d_skip=([128], F32),
        ssm_norm_g=([8192], F32), w_ssm_proj=([8192, D], F32), q_a_norm_g=([1024], F32), w_q_b=([1024, 12288], F32),
        kv_a_norm_g=([512], F32), w_kv_b=([512, 16384], F32), w_attn_proj=([8192, D], F32), w_out=([D, D], F32),
        ln1_g=([D], F32), ln1_b=([D], F32), w_router=([D, 64], F32), router_bias=([64], F32),
        w_exp_gate=([64, D, 768], F32), w_exp_up=([64, D, 768], F32), w_exp_down=([64, 768, D], F32),
        w_sh_gate=([D, 768], F32), w_sh_up=([D, 768], F32), w_sh_down=([768, D], F32), ln2_g=([D], F32), ln2_b=([D], F32))
    for n in names:
        sh, dt = shapes[n]
        setattr(C, n, nc.dram_tensor(n, list(sh), dt, kind="ExternalInput").ap())
    C.ar_t = P.sb("arena", [128, ARENA], F32)
    C.AR = Arena(C.ar_t, ARENA)
    C.pb = [P.ps("pb%d" % i, [128, 512], F32) for i in range(8)]
    C.rot = {}
    sc = lambda name, shape, dt=BF16: nc.dram_tensor(name, list(shape), dt).ap()
    C.kvagT_s = sc("kvagT_s", [512, L])
    C.krT_s = sc("krT_s", [128, L])
    C.cos_s = sc("cos_s", [128, L], F32)
    C.sin_s = sc("sin_s", [128, L], F32)
    C.qagT_s = sc("qagT_s", [1024, NQ])
    C.ynT_s = sc("ynT_s", [8192, NQ])
    C.attnT_s = sc("attnT_s", [8192, NQ])
    C.hT_s = sc("hT_s", [D, NQ])
    C.mixT_s = sc("mixT_s", [D, NQ])
    C.h1_s = sc("h1_s", [NQ, D], F32)
    C.h1T_s = sc("h1T_s", [D, NQ])
    C.wts_s = sc("wts_s", [NQ, 64], F32)
    return C


def bank(C, grp=(0, 1, 2, 3)):
    i = C.rot.get(grp, 0)
    C.rot[grp] = (i + 1) % len(grp)
    b = grp[i]
    return C.pb[b], "pb%d" % b


def tbank(C, grp=(6, 7)):
    i = C.rot.get(grp, 0)
    C.rot[grp] = (i + 1) % len(grp)
    b = grp[i]
    return C.pb[b][:, 0:512].bitcast(BF16), "pb%d" % b
def consts(C):
    P, A = C.P, C.AR
    C.it_i = A.i32(512)
    C.itf = A.f32(512)
    C.identb = A.bf16(128)
    C.triU = A.f32(128)
    C.SU = A.f32(128)
    C.ones_f = A.f32(128)
    C.ones_b = A.bf16(128)
    C.validt = A.f32(NT)
    C.g_in = A.f32(32); C.b_in = A.f32(32)
    C.kvg = A.f32(4); C.qg = A.f32(8)
    C.dtb_bc = A.f32(128); C.a_bc = A.f32(128); C.dsk_bc = A.f32(128)
    C.cw = A.f32(80 * 4); C.cb = A.f32(80)
    C.epsln = A.f32(1); C.epsrms = A.f32(1)
    C.invf = A.f32(1); C.sgn = A.f32(1); C.halfpi = A.f32(1)
    P.op("pool", lambda e: e.iota(C.it_i, pattern=[[1, 512]], base=0, channel_multiplier=-1), writes=["it_i"])
    P.op("dve", lambda e: e.tensor_copy(out=C.itf, in_=C.it_i), reads=["it_i"], writes=["itf"])
    P.op("dve", lambda e: e.tensor_single_scalar(out=C.identb, in_=C.itf[:, 0:128], scalar=0.0, op=ALU.is_equal), reads=["itf"], writes=["identb"])
    P.op("dve", lambda e: e.tensor_single_scalar(out=C.triU, in_=C.itf[:, 0:128], scalar=0.0, op=ALU.is_ge), reads=["itf"], writes=["triU"])
    P.op("dve", lambda e: e.tensor_single_scalar(out=C.SU, in_=C.itf[:, 0:128], scalar=0.0, op=ALU.is_lt), reads=["itf"], writes=["SU"])
    P.op("pool", lambda e: e.memset(C.ones_f, 1.0), writes=["ones_f"])
    P.op("pool", lambda e: e.memset(C.ones_b, 1.0), writes=["ones_b"])
    P.op("pool", lambda e: e.memset(C.epsln, EPS_LN), writes=["epsln"])
    P.op("pool", lambda e: e.memset(C.epsrms, EPS_RMS), writes=["epsrms"])
    P.op("pool", lambda e: e.memset(C.halfpi, math.pi / 2), writes=["halfpi"])
    P.dma("sp", C.validt, C.validtm, writes=["validt"])
    nck = dict(allow_slow_non_contiguous=True)
    P.dma("sp", C.g_in, C.ln_in_g.rearrange("(k p) -> p k", p=128), writes=["g_in"], **nck)
    P.dma("sp", C.b_in, C.ln_in_b.rearrange("(k p) -> p k", p=128), writes=["b_in"], **nck)
    P.dma("sp", C.kvg, C.kv_a_norm_g.rearrange("(k p) -> p k", p=128), writes=["kvg"], **nck)
    P.dma("sp", C.qg, C.q_a_norm_g.rearrange("(k p) -> p k", p=128), writes=["qg"], **nck)
    P.dma("sp", C.dtb_bc, C.dt_bias.partition_broadcast(128), writes=["dtb_bc"])
    P.dma("sp", C.a_bc, C.a_log.partition_broadcast(128), writes=["a_bc"])
    P.dma("sp", C.dsk_bc, C.d_skip.partition_broadcast(128), writes=["dsk_bc"])
    for k in range(4):
        P.dma("sp", C.cw.rearrange("p (m k) -> p m k", k=4)[:, :, k], C.conv_w[k].rearrange("(m p) -> p m", p=128), writes=["cw"], **nck)
    P.dma("sp", C.cb, C.conv_b.rearrange("(m p) -> p m", p=128), writes=["cb"], **nck)
    P.op("act", lambda e: e.activation(out=C.a_bc, in_=C.a_bc, func=AF.Exp), reads=["a_bc"], writes=["a_bc"])
    P.op("dve", lambda e: e.tensor_scalar_mul(out=C.a_bc, in0=C.a_bc, scalar1=-1.0), reads=["a_bc"], writes=["a_bc"])
    tmpi = A.i32(4)
    tmpf = A.f32(4)
    P.op("pool", lambda e: e.iota(tmpi[:, 0:1], pattern=[[0, 1]], base=0, channel_multiplier=1), writes=["tmpi"])
    P.op("dve", lambda e: e.tensor_single_scalar(out=tmpi[:, 1:2], in_=tmpi[:, 0:1], scalar=31, op=ALU.bitwise_and), reads=["tmpi"], writes=["tmpi1"])
    P.op("dve", lambda e: e.tensor_single_scalar(out=tmpi[:, 2:3], in_=tmpi[:, 0:1], scalar=32, op=ALU.bitwise_and), reads=["tmpi"], writes=["tmpi2"])
    P.op("dve", lambda e: e.tensor_copy(out=tmpf[:, 0:1], in_=tmpi[:, 1:2]), reads=["tmpi1"], writes=["tmpf0"])
    P.op("dve", lambda e: e.tensor_copy(out=tmpf[:, 1:2], in_=tmpi[:, 2:3]), reads=["tmpi2"], writes=["tmpf1"])
    P.op("act", lambda e: e.activation(out=C.invf, in_=tmpf[:, 0:1], func=AF.Exp, scale=-math.log(10000.0) / 32.0), reads=["tmpf0"], writes=["invf"])
    P.op("dve", lambda e: e.tensor_scalar(out=C.sgn, in0=tmpf[:, 1:2], scalar1=1.0 / 16.0, scalar2=-1.0, op0=ALU.mult, op1=ALU.add), reads=["tmpf1"], writes=["sgn"])
    C.base_off = A.off


def barrier(C):
    P = C.P
    toks = []
    for e in ENGS:
        for i in range(len(P.ops[e]) - 1, -1, -1):
            o = P.ops[e][i]
            if o["dma"] is None and o["fn"] is not None:
                toks.append(("eng", e, i))
                break
    dtoks = []
    for q in ("sp", "pool", "act"):
        for slot in range(NDMASEM):
            if P.dcnt[q][slot] > 0:
                dtoks.append(("dma", q, slot, 16 * P.dcnt[q][slot]))
    for f in ENGS:
        waits = []
        for t in toks:
            _, E, i = t
            if E == f:
                continue
            if P.waited[f].get(E, -1) >= i:
                continue
            P.waited[f][E] = i
            P.ops[E][i]["sig"] = True
            waits.append(t)
        for t in dtoks:
            _, q, slot, v = t
            if P.waited[f].get((q, slot), 0) >= v:
                continue
            P.waited[f][(q, slot)] = v
            waits.append(t)
        P.ops[f].append({"fn": None, "waits": waits, "sig": False, "dma": None})


def phase1(C, stop=None):
    P, A = C.P, C.AR
    w_in = C.w_in
    state = A.f32(8192).rearrange("p (g c) -> p g c", g=8)
    halo = A.f32(80 * 3).rearrange("p (m k) -> p m k", k=3)
    xt = A.f32(4096)
    xh = A.bf16(4096)
    hT = A.bf16(32 * 512).rearrange("p (k w) -> p k w", k=32)
    wb = [A.bf16(32 * 128).rearrange("p (k n) -> p k n", k=32) for _ in range(3)]
    wbn = [0]
    stats = A.f32(8 * 6); mv = A.f32(2); rstd = A.f32(1); nmr = A.f32(1)
    vbc = A.f32(512)
    dts = A.f32(4 * 128); dta = A.f32(4 * 128)
    raw = [A.f32(515) for _ in range(2)]
    rawn = [0]
    cacc = A.f32(512)
    xsT = A.bf16(8 * 512).rearrange("p (m w) -> p m w", m=8)
    BT = A.bf16(512); CT = A.bf16(512)
    offB = A.off
    kraw = A.f32(4 * 512).rearrange("p (m w) -> p m w", m=4)
    ksq = A.f32(512)
    rbc = A.f32(512)
    kout = A.bf16(8 * 512).rearrange("p (m w) -> p m w", m=8)
    posi = A.i32(512); posi2 = A.i32(512); ang = A.f32(512); kk = A.i32(512); kf = A.f32(512)
    cosT = A.f32(512); sinT = A.f32(512)
    endA = A.off
    A.off = offB
    xs_tm = A.bf16(1024); xdt = A.bf16(1024); xdtd = A.bf16(1024); B_tm = A.bf16(128)
    acs = A.f32(16); tmp16 = A.f32(16); dend = A.f32(16); wend = A.f32(16); cdec = A.f32(16); eacs = A.f32(16)
    prevb = A.bf16(1024)
    cbm = A.f32(128)
    lhs4 = A.f32(4 * 128).rearrange("p (h s) -> p h s", h=4)
    dec4 = A.f32(512)
    Mt = A.bf16(16 * 128).rearrange("p (h l) -> p h l", h=16)
    y1 = A.f32(1024); y2 = A.f32(1024)
    zs = A.bf16(4 * 1024).rearrange("p (t c) -> p t c", t=4)
    ng_bc = A.f32(1024)
    ss1 = A.f32(1); rs1 = A.f32(1)
    yn = A.bf16(1024)
    ynT = A.bf16(1024).rearrange("p (c t) -> p c t", c=8)
    A.off = max(A.off, endA)
    P.op("pool", lambda e: e.memset(state, 0.0), writes=["state"])
    P.op("pool", lambda e: e.memset(halo, 0.0), writes=["halo"])
    v3 = lambda ap: ap.rearrange("p (h d) -> p h d", h=16)
    b3 = lambda ap: ap.unsqueeze(2).broadcast_to([128, 16, 64])

    def load_w(src, col0, ncols):
        i = wbn[0]; wbn[0] = (i + 1) % 3
        key = "wb%d" % i
        P.dma("pool", wb[i][:, :, 0:ncols], src[:, col0:col0 + ncols].rearrange("(k p) n -> p k n", p=128), writes=[key])
        return wb[i], key

    sts = [(0, 1)] + [(1 + 4 * i, 4) for i in range(8)]
    for (t0, nt) in sts:
        W = 128 * nt
        s0 = 128 * t0
        isq = t0 >= QT0
        q0 = s0 - QT0 * 128
        for ti in range(nt):
            t = t0 + ti
            P.dma("sp", xt, C.xin[128 * t:128 * t + 128, :], writes=["xt"])
            for c in range(8):
                P.op("dve", lambda e, c=c: e.bn_stats(out=stats[:, 6 * c:6 * c + 6], in_=xt[:, 512 * c:512 * c + 512]), reads=["xt"], writes=["stats%d" % c])
            P.op("dve", lambda e: e.bn_aggr(out=mv, in_=stats), reads=["stats%d" % c for c in range(8)], writes=["mv"])
            P.op("act", lambda e: e.activation(out=rstd, in_=mv[:, 1:2], func=AF.Sqrt, bias=C.epsln, scale=1.0), reads=["mv", "epsln"], writes=["rstd"])
            P.op("dve", lambda e: e.reciprocal(out=rstd, in_=rstd), reads=["rstd"], writes=["rstd"])
            P.op("dve", lambda e: e.scalar_tensor_tensor(out=nmr, in0=mv[:, 0:1], scalar=-1.0, in1=rstd, op0=ALU.mult, op1=ALU.mult), reads=["mv", "rstd"], writes=["nmr"])
            P.op("act", lambda e: e.activation(out=xh, in_=xt, func=AF.Identity, scale=rstd, bias=nmr), reads=["xt", "rstd", "nmr"], writes=["xh"])
            for k8 in range(4):
                pt, pk = tbank(C)
                for j in range(8):
                    kc = 8 * k8 + j
                    P.op("pe", lambda e, pt=pt, j=j, kc=kc: e.transpose(out=pt[:, 128 * j:128 * j + 128], in_=xh[:, 128 * kc:128 * kc + 128], identity=C.identb),
                         reads=["xh", "identb"], writes=[pk])
                for j in range(8):
                    kc = 8 * k8 + j
                    if j % 2 == 0:
                        P.op("dve", lambda e, pt=pt, j=j, kc=kc, ti=ti: e.tensor_scalar(out=hT[:, kc, 128 * ti:128 * ti + 128], in0=pt[:, 128 * j:128 * j + 128],
                                                                                  scalar1=C.g_in[:, kc:kc + 1], scalar2=C.b_in[:, kc:kc + 1], op0=ALU.mult, op1=ALU.add),
                             reads=[pk, "g_in", "b_in"], writes=["hT"])
                    else:
                        P.op("act", lambda e, pt=pt, j=j, kc=kc, ti=ti: e.activation(out=hT[:, kc, 128 * ti:128 * ti + 128], in_=pt[:, 128 * j:128 * j + 128], func=AF.Identity,
                                                                               scale=C.g_in[:, kc:kc + 1], bias=C.b_in[:, kc:kc + 1]),
                             reads=[pk, "g_in", "b_in"], writes=["hT"])
        if isq:
            P.dma("sp", C.hT_s.rearrange("(k p) t -> p k t", p=128)[:, :, q0:q0 + W], hT[:, :, 0:W], reads=["hT"], writes=["hT_s"])
        P.dma("sp", vbc[:, 0:W], C.validrow[0:1, s0:s0 + W].partition_broadcast(128), writes=["vbc"])

        def fm(col0, nm, epi):
            for m in range(nm):
                wbuf, wkey = load_w(w_in, col0 + 128 * m, 128)
                pbk, pkey = bank(C)
                for kc in range(32):
                    P.op("pe", lambda e, pbk=pbk, wbuf=wbuf, kc=kc: e.matmul(pbk[:, 0:W], lhsT=wbuf[:, kc, 0:128], rhs=hT[:, kc, 0:W], start=(kc == 0), stop=(kc == 31)),
                         reads=[wkey, "hT"], writes=[pkey])
                epi(m, pbk, pkey)

        wbuf, wkey = load_w(w_in, ODT, 128)
        for ti in range(nt):
            pbk, pkey = bank(C)
            for kc in range(32):
                P.op("pe", lambda e, pbk=pbk, wbuf=wbuf, kc=kc, ti=ti: e.matmul(pbk[:, 0:128], lhsT=hT[:, kc, 128 * ti:128 * ti + 128], rhs=wbuf[:, kc, 0:128],
                                                                             start=(kc == 0), stop=(kc == 31)), reads=[wkey, "hT"], writes=[pkey])
            d = dts[:, 128 * ti:128 * ti + 128]
            dk = "dts%d" % ti
            P.op("dve", lambda e, d=d, pbk=pbk: e.tensor_tensor(out=d, in0=pbk[:, 0:128], in1=C.dtb_bc, op=ALU.add), reads=[pkey, "dtb_bc"], writes=[dk])
            P.op("act", lambda e, d=d: e.activation(out=d, in_=d, func=AF.Exp), reads=[dk], writes=[dk])
            P.op("act", lambda e, d=d: e.activation(out=d, in_=d, func=AF.Ln, bias=1.0), reads=[dk], writes=[dk])
            P.op("dve", lambda e, d=d, ti=ti: e.tensor_tensor(out=dta[:, 128 * ti:128 * ti + 128], in0=d, in1=C.a_bc, op=ALU.mult), reads=[dk, "a_bc"], writes=["dta%d" % ti])

        def epi_kv(mi, pbk, pkey):
            P.op("dve", lambda e: e.tensor_tensor(out=kraw[:, mi, 0:W], in0=pbk[:, 0:W], in1=vbc[:, 0:W], op=ALU.mult), reads=[pkey, "vbc"], writes=["kraw%d" % mi])
        fm(OKVA, 4, epi_kv)
        pbk, pkey = bank(C, (4,))
        for mi in range(4):
            P.op("act", lambda e, mi=mi: e.activation(out=ksq[:, 0:W], in_=kraw[:, mi, 0:W], func=AF.Square), reads=["kraw%d" % mi], writes=["ksq"])
            P.op("pe", lambda e, mi=mi, pbk=pbk: e.matmul(pbk[:, 0:W], lhsT=C.ones_f, rhs=ksq[:, 0:W], start=(mi == 0), stop=(mi == 3)), reads=["ksq", "ones_f"], writes=[pkey])
        P.op("act", lambda e, pbk=pbk: e.activation(out=rbc[:, 0:W], in_=pbk[:, 0:W], func=AF.Sqrt, bias=C.epsrms, scale=1.0 / 512.0), reads=[pkey, "epsrms"], writes=["rbc"])
        P.op("dve", lambda e: e.reciprocal(out=rbc[:, 0:W], in_=rbc[:, 0:W]), reads=["rbc"], writes=["rbc"])
        for mi in range(4):
            P.op("dve", lambda e, mi=mi: e.scalar_tensor_tensor(out=kout[:, mi, 0:W], in0=kraw[:, mi, 0:W], scalar=C.kvg[:, mi:mi + 1], in1=rbc[:, 0:W], op0=ALU.mult, op1=ALU.mult),
                 reads=["kraw%d" % mi, "rbc", "kvg"], writes=["kout"])
        P.dma("sp", C.kvagT_s.rearrange("(k p) t -> p k t", p=128)[:, :, s0:s0 + W], kout[:, 0:4, 0:W], reads=["kout"], writes=["kvagT_s"])

        P.dma("sp", posi[:, 0:W], C.posraw[0:1, s0:s0 + W].partition_broadcast(128), writes=["posi"])
        P.dma("sp", posi2[:, 0:W], C.posoff[0:1, s0:s0 + W].partition_broadcast(128), writes=["posi2"])
        P.op("dve", lambda e: e.tensor_copy(out=ang[:, 0:W], in_=posi[:, 0:W]), reads=["posi"], writes=["ang"])
        P.op("dve", lambda e: e.tensor_copy(out=kf[:, 0:W], in_=posi2[:, 0:W]), reads=["posi2"], writes=["kf"])
        P.op("dve", lambda e: e.tensor_tensor(out=ang[:, 0:W], in0=ang[:, 0:W], in1=kf[:, 0:W], op=ALU.add), reads=["ang", "kf"], writes=["ang"])
        P.op("dve", lambda e: e.tensor_scalar_mul(out=ang[:, 0:W], in0=ang[:, 0:W], scalar1=C.invf), reads=["ang", "invf"], writes=["ang"])

        def sincos(outt, okey, shift, scale_ap):
            P.op("dve", lambda e: e.tensor_scalar(out=kf[:, 0:W], in0=ang[:, 0:W], scalar1=shift, scalar2=1.0 / (2 * math.pi), op0=ALU.add, op1=ALU.mult), reads=["ang"], writes=["kf"])
            P.op("dve", lambda e: e.tensor_copy(out=kk[:, 0:W], in_=kf[:, 0:W]), reads=["kf"], writes=["kk"])
            P.op("dve", lambda e: e.tensor_copy(out=kf[:, 0:W], in_=kk[:, 0:W]), reads=["kk"], writes=["kf"])
            P.op("dve", lambda e: e.scalar_tensor_tensor(out=kf[:, 0:W], in0=kf[:, 0:W], scalar=-2 * math.pi, in1=ang[:, 0:W], op0=ALU.mult, op1=ALU.add), reads=["kf", "ang"], writes=["kf"])
            P.op("dve", lambda e: e.tensor_scalar(out=kf[:, 0:W], in0=kf[:, 0:W], scalar1=shift, scalar2=-3.14159, op0=ALU.add, op1=ALU.max), reads=["kf"], writes=["kf"])
            P.op("dve", lambda e: e.tensor_scalar_min(out=kf[:, 0:W], in0=kf[:, 0:W], scalar1=3.14159), reads=["kf"], writes=["kf"])
            if scale_ap is None:
                P.op("act", lambda e: e.activation(out=outt[:, 0:W], in_=kf[:, 0:W], func=AF.Sin), reads=["kf"], writes=[okey])
            else:
                P.op("act", lambda e: e.activation(out=outt[:, 0:W], in_=kf[:, 0:W], func=AF.Sin, scale=scale_ap), reads=["kf", "sgn"], writes=[okey])
        sincos(cosT, "cosT", math.pi / 2, None)
        sincos(sinT, "sinT", 0.0, C.sgn)
        P.dma("sp", C.cos_s[:, s0:s0 + W], cosT[:, 0:W], reads=["cosT"], writes=["cos_s"])
        P.dma("sp", C.sin_s[:, s0:s0 + W], sinT[:, 0:W], reads=["sinT"], writes=["sin_s"])

        wr = w_in[:, OKR:OKR + 64].rearrange("(k p) n -> p k n", p=128)
        pks = []
        for lay in (((0, 0, 64), (64, 0, 64)), ((0, 32, 32), (32, 0, 32), (64, 32, 32), (96, 0, 32))):
            i = wbn[0]; wbn[0] = (i + 1) % 3
            wkey = "wb%d" % i
            wbuf = wb[i]
            for (dst, src0, n) in lay:
                P.dma("pool", wbuf[:, :, dst:dst + n], wr[:, :, src0:src0 + n], writes=[wkey])
            pbk, pkey = bank(C)
            for kc in range(32):
                P.op("pe", lambda e, pbk=pbk, wbuf=wbuf, kc=kc: e.matmul(pbk[:, 0:W], lhsT=wbuf[:, kc, 0:128], rhs=hT[:, kc, 0:W], start=(kc == 0), stop=(kc == 31)),
                     reads=[wkey, "hT"], writes=[pkey])
            pks.append((pbk, pkey))
        (pk1, pk1k), (pk2, pk2k) = pks
        krt = kraw[:, 0, :]
        P.op("dve", lambda e: e.tensor_tensor(out=krt[:, 0:W], in0=pk1[:, 0:W], in1=cosT[:, 0:W], op=ALU.mult), reads=[pk1k, "cosT", "kraw0"], writes=["kraw0"])
        P.op("dve", lambda e: e.tensor_tensor(out=ksq[:, 0:W], in0=pk2[:, 0:W], in1=sinT[:, 0:W], op=ALU.mult), reads=[pk2k, "sinT"], writes=["ksq"])
        P.op("dve", lambda e: e.tensor_tensor(out=krt[:, 0:W], in0=krt[:, 0:W], in1=ksq[:, 0:W], op=ALU.add), reads=["kraw0", "ksq"], writes=["kraw0"])
        P.op("dve", lambda e: e.tensor_tensor(out=kout[:, 4, 0:W], in0=krt[:, 0:W], in1=vbc[:, 0:W], op=ALU.mult), reads=["kraw0", "vbc"], writes=["kout"])
        P.dma("sp", C.krT_s[:, s0:s0 + W], kout[:, 4, 0:W], reads=["kout"], writes=["krT_s"])

        if isq:
            qss, qssk = C.pb[5], "pb5"
            for half in range(2):
                def epi_q(mi, pbk, pkey):
                    P.op("act", lambda e: e.activation(out=kraw[:, mi, 0:W], in_=pbk[:, 0:W], func=AF.Identity), reads=[pkey], writes=["kraw%d" % mi])
                fm(OQA + 512 * half, 4, epi_q)
                for mi in range(4):
                    P.op("act", lambda e, mi=mi: e.activation(out=ksq[:, 0:W], in_=kraw[:, mi, 0:W], func=AF.Square), reads=["kraw%d" % mi], writes=["ksq"])
                    P.op("pe", lambda e, mi=mi, half=half: e.matmul(qss[:, 0:W], lhsT=C.ones_f, rhs=ksq[:, 0:W], start=(half == 0 and mi == 0), stop=(half == 1 and mi == 3)),
                         reads=["ksq", "ones_f"], writes=[qssk])
                for mi in range(4):
                    P.op("dve", lambda e, mi=mi, half=half: e.tensor_scalar_mul(out=xsT[:, 4 * half + mi, 0:W], in0=kraw[:, mi, 0:W], scalar1=C.qg[:, 4 * half + mi:4 * half + mi + 1]),
                         reads=["kraw%d" % mi, "qg"], writes=["xsT"])
            P.op("act", lambda e: e.activation(out=rbc[:, 0:W], in_=qss[:, 0:W], func=AF.Sqrt, bias=C.epsrms, scale=1.0 / 1024.0), reads=[qssk, "epsrms"], writes=["rbc"])
            P.op("dve", lambda e: e.reciprocal(out=rbc[:, 0:W], in_=rbc[:, 0:W]), reads=["rbc"], writes=["rbc"])
            for mi in range(8):
                P.op("dve", lambda e, mi=mi: e.tensor_tensor(out=kout[:, mi, 0:W], in0=xsT[:, mi, 0:W], in1=rbc[:, 0:W], op=ALU.mult), reads=["xsT", "rbc"], writes=["kout"])
            P.dma("sp", C.qagT_s.rearrange("(k p) t -> p k t", p=128)[:, :, q0:q0 + W], kout[:, :, 0:W], reads=["kout"], writes=["qagT_s"])
        barrier(C)
        if stop == "kv" and t0 == 1:
            return

        for g in range(8):
            def epi_conv(mi, pbk, pkey, g=g):
                ch = 8 * g + mi if mi < 8 else (64 + g if mi == 8 else 72 + g)
                ri = rawn[0]; rawn[0] = 1 - ri
                rb, rk = raw[ri], "raw%d" % ri
                P.op("pool", lambda e: e.tensor_copy(out=rb[:, 0:3], in_=halo[:, ch, :]), reads=["halo", "halo%d" % ch], writes=[rk])
                P.op("dve", lambda e: e.tensor_tensor(out=rb[:, 3:3 + W], in0=pbk[:, 0:W], in1=vbc[:, 0:W], op=ALU.mult), reads=[pkey, "vbc", rk], writes=[rk])
                P.op("pool", lambda e: e.tensor_copy(out=halo[:, ch, :], in_=rb[:, W:W + 3]), reads=[rk], writes=["halo%d" % ch])
                P.op("pool", lambda e: e.tensor_scalar_mul(out=cacc[:, 0:W], in0=rb[:, 0:W], scalar1=C.cw[:, 4 * ch:4 * ch + 1]), reads=[rk, "cw"], writes=["cacc"])
                for k in range(1, 4):
                    P.op("dve", lambda e, k=k: e.scalar_tensor_tensor(out=cacc[:, 0:W], in0=rb[:, k:k + W], scalar=C.cw[:, 4 * ch + k:4 * ch + k + 1], in1=cacc[:, 0:W], op0=ALU.mult, op1=ALU.add),
                         reads=[rk, "cw", "cacc"], writes=["cacc"])
                dst, dk = (xsT[:, mi, 0:W], "xsT") if mi < 8 else ((BT[:, 0:W], "BT") if mi == 8 else (CT[:, 0:W], "CT"))
                P.op("act", lambda e: e.activation(out=dst, in_=cacc[:, 0:W], func=AF.Silu, bias=C.cb[:, ch:ch + 1]), reads=["cacc", "cb"], writes=[dk])

            def fm_cols(cols, epi):
                for mi, c0 in enumerate(cols):
                    wbuf, wkey = load_w(w_in, c0, 128)
                    pbk, pkey = bank(C)
                    for kc in range(32):
                        P.op("pe", lambda e, pbk=pbk, wbuf=wbuf, kc=kc: e.matmul(pbk[:, 0:W], lhsT=wbuf[:, kc, 0:128], rhs=hT[:, kc, 0:W], start=(kc == 0), stop=(kc == 31)),
                             reads=[wkey, "hT"], writes=[pkey])
                    epi(mi, pbk, pkey)
            fm_cols([OX + 1024 * g + 128 * m for m in range(8)] + [OB + 128 * g, OC + 128 * g], epi_conv)
            if isq:
                P.dma("sp", ng_bc, C.ssm_norm_g[1024 * g:1024 * g + 1024].partition_broadcast(128), writes=["ng_bc"])
                for cbk in range(8):
                    wbuf, wkey = load_w(w_in, OZ + 1024 * g + 128 * cbk, 128)
                    for ti in range(nt):
                        pbk, pkey = bank(C)
                        for kc in range(32):
                            P.op("pe", lambda e, pbk=pbk, wbuf=wbuf, kc=kc, ti=ti: e.matmul(pbk[:, 0:128], lhsT=hT[:, kc, 128 * ti:128 * ti + 128], rhs=wbuf[:, kc, 0:128],
                                                                                         start=(kc == 0), stop=(kc == 31)), reads=[wkey, "hT"], writes=[pkey])
                        P.op("act", lambda e, pbk=pbk, ti=ti, cbk=cbk: e.activation(out=zs[:, ti, 128 * cbk:128 * cbk + 128], in_=pbk[:, 0:128], func=AF.Silu), reads=[pkey], writes=["zs"])
            for ti in range(nt):
                t = t0 + ti
                cs = slice(128 * ti, 128 * ti + 128)
                isq_t = t >= QT0
                dta_g = dta[:, 128 * ti + 16 * g:128 * ti + 16 * g + 16]
                dt_g = dts[:, 128 * ti + 16 * g:128 * ti + 16 * g + 16]
                pt, pk = tbank(C)
                for m in range(8):
                    P.op("pe", lambda e, pt=pt, m=m: e.transpose(out=pt[:, 128 * m:128 * m + 128], in_=xsT[:, m, cs], identity=C.identb), reads=["xsT", "identb"], writes=[pk])
                P.op("dve", lambda e, pt=pt, t=t: e.tensor_scalar_mul(out=xs_tm, in0=pt[:, 0:1024], scalar1=C.validt[:, t:t + 1]), reads=[pk, "validt"], writes=["xs_tm"])
                pt2, pk2 = tbank(C)
                P.op("pe", lambda e, pt2=pt2: e.transpose(out=pt2[:, 0:128], in_=BT[:, cs], identity=C.identb), reads=["BT", "identb"], writes=[pk2])
                P.op("act", lambda e, pt2=pt2: e.activation(out=B_tm, in_=pt2[:, 0:128], func=AF.Identity), reads=[pk2], writes=["B_tm"])
                pa, pak = bank(C, (4,))
                P.op("pe", lambda e, pa=pa: e.matmul(pa[:, 0:16], lhsT=C.triU, rhs=dta_g, start=True, stop=True), reads=["triU", "dta%d" % ti], writes=[pak])
                P.op("pe", lambda e, pa=pa: e.matmul(pa[:, 16:32], lhsT=C.ones_f, rhs=dta_g, start=True, stop=True), reads=["ones_f", "dta%d" % ti], writes=[pak])
                P.op("act", lambda e, pa=pa: e.activation(out=acs, in_=pa[:, 0:16], func=AF.Identity), reads=[pak], writes=["acs"])
                P.op("dve", lambda e, pa=pa: e.tensor_tensor(out=tmp16, in0=pa[:, 16:32], in1=acs, op=ALU.subtract), reads=[pak, "acs"], writes=["tmp16"])
                P.op("act", lambda e: e.activation(out=dend, in_=tmp16, func=AF.Exp), reads=["tmp16"], writes=["dend"])
                P.op("dve", lambda e: e.tensor_tensor(out=wend, in0=dend, in1=dt_g, op=ALU.mult), reads=["dend", "dts%d" % ti], writes=["wend"])
                P.op("act", lambda e, pa=pa: e.activation(out=cdec, in_=pa[:, 16:32], func=AF.Exp), reads=[pak], writes=["cdec"])
                P.op("dve", lambda e: e.tensor_tensor(out=v3(xdtd), in0=v3(xs_tm), in1=b3(wend), op=ALU.mult), reads=["xs_tm", "wend"], writes=["xdtd"])
                if isq_t:
                    P.op("pool", lambda e, g=g: e.tensor_copy(out=prevb, in_=state[:, g, :]), reads=["state", "state%d" % g], writes=["prevb"])
                pn = [bank(C, (0, 1, 2, 3)) for _ in range(2)]
                for hf in range(2):
                    P.op("pe", lambda e, hf=hf: e.matmul(pn[hf][0][:, 0:512], lhsT=B_tm, rhs=xdtd[:, 512 * hf:512 * hf + 512], start=True, stop=True), reads=["B_tm", "xdtd"], writes=[pn[hf][1]])
                P.op("dve", lambda e, g=g: e.tensor_tensor(out=v3(state[:, g, :]), in0=v3(state[:, g, :]), in1=b3(cdec), op=ALU.mult), reads=["state", "state%d" % g, "cdec", "prevb"], writes=["state%d" % g])
                for hf in range(2):
                    P.op("dve", lambda e, g=g, hf=hf: e.tensor_tensor(out=state[:, g, 512 * hf:512 * hf + 512], in0=state[:, g, 512 * hf:512 * hf + 512], in1=pn[hf][0][:, 0:512], op=ALU.add),
                         reads=["state%d" % g, pn[hf][1]], writes=["state%d" % g])
                if not isq_t:
                    continue
                P.op("act", lambda e, pa=pa: e.activation(out=eacs, in_=pa[:, 0:16], func=AF.Exp), reads=[pak], writes=["eacs"])
                pc, pck = bank(C, (5,))
                P.op("pe", lambda e, pc=pc: e.matmul(pc[:, 0:128], lhsT=BT[:, cs], rhs=CT[:, cs], start=True, stop=True), reads=["BT", "CT"], writes=[pck])
                P.op("dve", lambda e, pc=pc: e.tensor_tensor(out=cbm, in0=pc[:, 0:128], in1=C.triU, op=ALU.mult), reads=[pck, "triU"], writes=["cbm"])
                for q4 in range(4):
                    for hh in range(4):
                        P.op("pool", lambda e, hh=hh, q4=q4: e.tensor_scalar_mul(out=lhs4[:, hh, :], in0=C.SU, scalar1=dta_g[:, 4 * q4 + hh:4 * q4 + hh + 1]), reads=["SU", "dta%d" % ti], writes=["lhs4_%d" % hh])
                    pd, pdk = bank(C, (0, 1, 2, 3))
                    for hh in range(4):
                        P.op("pe", lambda e, pd=pd, hh=hh: e.matmul(pd[:, 128 * hh:128 * hh + 128], lhsT=lhs4[:, hh, :], rhs=C.triU, start=True, stop=True), reads=["lhs4_%d" % hh, "triU"], writes=[pdk])
                    P.op("act", lambda e, pd=pd: e.activation(out=dec4, in_=pd[:, 0:512], func=AF.Exp), reads=[pdk], writes=["dec4"])
                    P.op("dve", lambda e, q4=q4: e.tensor_tensor(out=Mt[:, 4 * q4:4 * q4 + 4, :], in0=dec4.rearrange("p (h l) -> p h l", h=4), in1=cbm.unsqueeze(1).broadcast_to([128, 4, 128]), op=ALU.mult),
                         reads=["dec4", "cbm"], writes=["Mt"])
                P.op("pool", lambda e: e.tensor_tensor(out=v3(xdt), in0=v3(xs_tm), in1=b3(dt_g), op=ALU.mult), reads=["xs_tm", "dts%d" % ti], writes=["xdt"])
                py = [bank(C, (0, 1, 2, 3)) for _ in range(2)]
                for h in range(16):
                    P.op("pe", lambda e, h=h: e.matmul(py[h // 8][0][:, 64 * (h % 8):64 * (h % 8) + 64], lhsT=Mt[:, h, :], rhs=xdt[:, 64 * h:64 * h + 64], start=True, stop=True),
                         reads=["Mt", "xdt"], writes=[py[h // 8][1]])
                po = [bank(C, (0, 1, 2, 3)) for _ in range(2)]
                for hf in range(2):
                    P.op("pe", lambda e, hf=hf: e.matmul(po[hf][0][:, 0:512], lhsT=CT[:, cs], rhs=prevb[:, 512 * hf:512 * hf + 512], start=True, stop=True), reads=["CT", "prevb"], writes=[po[hf][1]])
                for hf in range(2):
                    hs = slice(512 * hf, 512 * hf + 512)
                    v8 = lambda ap: ap.rearrange("p (h d) -> p h d", h=8)
                    P.op("dve", lambda e, hf=hf, hs=hs: e.tensor_tensor(out=v8(y1[:, hs]), in0=v8(po[hf][0][:, 0:512]), in1=eacs[:, 8 * hf:8 * hf + 8].unsqueeze(2).broadcast_to([128, 8, 64]), op=ALU.mult),
                         reads=[po[hf][1], "eacs"], writes=["y1_%d" % hf])
                    P.op("dve", lambda e, hf=hf, hs=hs: e.tensor_tensor(out=y1[:, hs], in0=y1[:, hs], in1=py[hf][0][:, 0:512], op=ALU.add), reads=["y1_%d" % hf, py[hf][1]], writes=["y1_%d" % hf])
                P.op("pool", lambda e, g=g: e.tensor_tensor(out=v3(y2), in0=v3(xs_tm), in1=b3(C.dsk_bc[:, 16 * g:16 * g + 16]), op=ALU.mult), reads=["xs_tm", "dsk_bc"], writes=["y2"])
                P.op("dve", lambda e: e.tensor_tensor(out=y1, in0=y1, in1=y2, op=ALU.add), reads=["y1_0", "y1_1", "y2"], writes=["y1_0", "y1_1"])
                P.op("dve", lambda e, ti=ti: e.tensor_tensor(out=y1, in0=y1, in1=zs[:, ti, :], op=ALU.mult), reads=["y1_0", "y1_1", "zs"], writes=["y1_0", "y1_1"])
                P.op("act", lambda e: e.activation(out=y2, in_=y1, func=AF.Square, accum_out=ss1), reads=["y1_0", "y1_1", "y2"], writes=["y2", "ss1"])
                P.op("act", lambda e: e.activation(out=rs1, in_=ss1, func=AF.Sqrt, bias=C.epsrms, scale=1.0 / 1024.0), reads=["ss1", "epsrms"], writes=["rs1"])
                P.op("dve", lambda e: e.reciprocal(out=rs1, in_=rs1), reads=["rs1"], writes=["rs1"])
                P.op("dve", lambda e: e.scalar_tensor_tensor(out=yn, in0=y1, scalar=rs1, in1=ng_bc, op0=ALU.mult, op1=ALU.mult), reads=["y1_0", "y1_1", "rs1", "ng_bc"], writes=["yn"])
                pt3, pk3 = tbank(C)
                for c in range(8):
                    P.op("pe", lambda e, pt3=pt3, c=c: e.transpose(out=pt3[:, 128 * c:128 * c + 128], in_=yn[:, 128 * c:128 * c + 128], identity=C.identb), reads=["yn", "identb"], writes=[pk3])
                P.op("act", lambda e, pt3=pt3: e.activation(out=ynT.rearrange("p c t -> p (c t)"), in_=pt3[:, 0:1024], func=AF.Identity), reads=[pk3], writes=["ynT"])
                qs = 128 * (t - QT0)
                P.dma("sp", C.ynT_s.rearrange("(g c p) t -> g p c t", g=8, p=128)[g][:, :, qs:qs + 128], ynT, reads=["ynT"], writes=["ynT_s"])
        barrier(C)
        if stop == "ssd" and t0 == 1:
            return


def attention(C, stop=None):
    P, A = C.P, C.AR
    A.off = C.base_off
    SC = 192.0 ** -0.5
    kvag = A.bf16(4 * L).rearrange("p (k w) -> p k w", k=4)
    krT = A.bf16(L)
    vones = A.bf16(NT * 128).rearrange("p (t d) -> p t d", t=NT)
    cm = A.bf16(4 * 512).rearrange("p (o q) -> p o q", o=4)
    cosq = A.f32(NQ); sinq = A.f32(NQ)
    wk = A.bf16(4 * 128).rearrange("p (k n) -> p k n", k=4)
    wv = A.bf16(4 * 128).rearrange("p (k n) -> p k n", k=4)
    wq = A.bf16(8 * 128).rearrange("p (k n) -> p k n", k=8)
    wqr = A.bf16(8 * 64).rearrange("p (k n) -> p k n", k=8)
    wqs = A.bf16(8 * 64).rearrange("p (k n) -> p k n", k=8)
    KT = A.bf16(L)
    V = A.bf16(NT * 128).rearrange("p (t d) -> p t d", t=NT)
    qag = [A.bf16(8 * 512).rearrange("p (k w) -> p k w", k=8) for _ in range(2)]
    QT = A.bf16(512); Qr = A.bf16(512)
    ta = A.f32(512); tb = A.f32(512)
    PT = [A.bf16(512) for _ in range(3)]
    rsb = A.f32(512)
    oT = [A.bf16(512) for _ in range(2)]
    P.dma("sp", kvag, C.kvagT_s.rearrange("(k p) t -> p k t", p=128), reads=["kvagT_s"], writes=["kvag"])
    P.dma("sp", krT[0:64, :], C.krT_s[0:64, :], reads=["krT_s"], writes=["krT"])
    P.dma("sp", cosq[0:64, :], C.cos_s[0:64, QT0 * 128:], reads=["cos_s"], writes=["cosq"])
    P.dma("sp", sinq[0:64, :], C.sin_s[0:64, QT0 * 128:], reads=["sin_s"], writes=["sinq"])
    for o in range(4):
        P.op("dve", lambda e, o=o: e.tensor_single_scalar(out=cm[:, o, :], in_=C.itf, scalar=128.0 * o, op=ALU.is_ge), reads=["itf"], writes=["cm"])
    for t in range(NT):
        P.op("dve", lambda e, t=t: e.tensor_scalar_mul(out=vones[:, t, :], in0=C.ones_b, scalar1=C.validt[:, t:t + 1]), reads=["ones_b", "validt"], writes=["vones"])
    nheads = 64 if stop != "attn1" else 1
    qn = 0
    for h in range(nheads):
        kvsrc = C.w_kv_b[:, 256 * h:256 * h + 256].rearrange("(k p) n -> p k n", p=128)
        P.dma("pool", wk, kvsrc[:, :, 0:128], writes=["wk"])
        P.dma("pool", wv, kvsrc[:, :, 128:256], writes=["wv"])
        qsrc = C.w_q_b[:, 192 * h:192 * h + 192].rearrange("(k p) n -> p k n", p=128)
        P.dma("pool", wq, qsrc[:, :, 0:128], writes=["wq"])
        P.dma("pool", wqr, qsrc[:, :, 128:192], writes=["wqr"])
        P.dma("pool", wqs[:, :, 0:32], qsrc[:, :, 160:192], writes=["wqs"])
        P.dma("pool", wqs[:, :, 32:64], qsrc[:, :, 128:160], writes=["wqs"])
        for n0 in range(0, L, 512):
            w_ = min(512, L - n0)
            pbk, pkey = bank(C, (6, 7))
            for kc in range(4):
                P.op("pe", lambda e, pbk=pbk, kc=kc, n0=n0, w_=w_: e.matmul(pbk[:, 0:w_], lhsT=wk[:, kc, :], rhs=kvag[:, kc, n0:n0 + w_], start=(kc == 0), stop=(kc == 3)),
                     reads=["wk", "kvag"], writes=[pkey])
            eng = "act" if (n0 // 512) % 2 == 0 else "dve"
            if eng == "act":
                P.op("act", lambda e, pbk=pbk, n0=n0, w_=w_: e.activation(out=KT[:, n0:n0 + w_], in_=pbk[:, 0:w_], func=AF.Identity), reads=[pkey], writes=["KT"])
            else:
                P.op("dve", lambda e, pbk=pbk, n0=n0, w_=w_: e.tensor_copy(out=KT[:, n0:n0 + w_], in_=pbk[:, 0:w_]), reads=[pkey], writes=["KT"])
        for t in range(NT):
            if t % 4 == 0:
                pbk, pkey = bank(C, (6, 7))
            c0 = 128 * (t % 4)
            for kc in range(4):
                P.op("pe", lambda e, pbk=pbk, kc=kc, t=t, c0=c0: e.matmul(pbk[:, c0:c0 + 128], lhsT=kvag[:, kc, 128 * t:128 * t + 128], rhs=wv[:, kc, :], start=(kc == 0), stop=(kc == 3)),
                     reads=["wv", "kvag"], writes=[pkey])
            P.op("dve", lambda e, pbk=pbk, t=t, c0=c0: e.tensor_scalar_mul(out=V[:, t, :], in0=pbk[:, c0:c0 + 128], scalar1=C.validt[:, t:t + 1]), reads=[pkey, "validt"], writes=["V"])
        for Q in range(NQ // 512):
            qc = slice(512 * Q, 512 * Q + 512)
            qi = qn % 2; qn += 1
            qb, qk = qag[qi], "qag%d" % qi
            P.dma("sp", qb, C.qagT_s.rearrange("(k p) t -> p k t", p=128)[:, :, qc], reads=["qagT_s"], writes=[qk])
            pbk, pkey = bank(C, (6, 7))
            for kc in range(8):
                P.op("pe", lambda e, pbk=pbk, kc=kc, qb=qb: e.matmul(pbk[:, 0:512], lhsT=wq[:, kc, :], rhs=qb[:, kc, :], start=(kc == 0), stop=(kc == 7)), reads=["wq", qk], writes=[pkey])
            P.op("act", lambda e, pbk=pbk: e.activation(out=QT, in_=pbk[:, 0:512], func=AF.Identity), reads=[pkey], writes=["QT"])
            p1, p1k = bank(C, (6, 7))
            for kc in range(8):
                P.op("pe", lambda e, p1=p1, kc=kc, qb=qb: e.matmul(p1[0:64, 0:512], lhsT=wqr[:, kc, :], rhs=qb[:, kc, :], start=(kc == 0), stop=(kc == 7)), reads=["wqr", qk], writes=[p1k])
            P.op("dve", lambda e, p1=p1, qc=qc: e.tensor_tensor(out=ta[0:64, :], in0=p1[0:64, 0:512], in1=cosq[0:64, qc], op=ALU.mult), reads=[p1k, "cosq"], writes=["ta"])
            p2, p2k = bank(C, (6, 7))
            for kc in range(8):
                P.op("pe", lambda e, p2=p2, kc=kc, qb=qb: e.matmul(p2[0:64, 0:512], lhsT=wqs[:, kc, :], rhs=qb[:, kc, :], start=(kc == 0), stop=(kc == 7)), reads=["wqs", qk], writes=[p2k])
            P.op("dve", lambda e, p2=p2, qc=qc: e.tensor_tensor(out=tb[0:64, :], in0=p2[0:64, 0:512], in1=sinq[0:64, qc], op=ALU.mult), reads=[p2k, "sinq"], writes=["tb"])
            P.op("dve", lambda e: e.tensor_tensor(out=Qr[0:64, :], in0=ta[0:64, :], in1=tb[0:64, :], op=ALU.add), reads=["ta", "tb"], writes=["Qr"])
            TQ = QT0 + 4 * Q
            jl = TQ + 3
            po, pok = bank(C, (2, 3))
            pr, prk = bank(C, (4, 5))
            for j in range(jl + 1):
                ks = slice(128 * j, 128 * j + 128)
                ps_, psk = bank(C, (0, 1))
                P.op("pe", lambda e, ps_=ps_, ks=ks: e.matmul(ps_[:, 0:512], lhsT=KT[:, ks], rhs=QT, start=True, stop=False), reads=["KT", "QT"], writes=[psk])
                P.op("pe", lambda e, ps_=ps_, ks=ks: e.matmul(ps_[:, 0:512], lhsT=krT[0:64, ks], rhs=Qr[0:64, :], start=False, stop=True), reads=["krT", "Qr"], writes=[psk])
                pi = (j % 3)
                ptb, ptk = PT[pi], "PT%d" % pi
                P.op("act", lambda e, ps_=ps_, ptb=ptb: e.activation(out=ptb, in_=ps_[:, 0:512], func=AF.Exp, scale=SC), reads=[psk], writes=[ptk])
                if j >= TQ:
                    P.op("pool", lambda e, ptb=ptb, o=j - TQ: e.tensor_tensor(out=ptb, in0=ptb, in1=cm[:, o, :], op=ALU.mult), reads=[ptk, "cm"], writes=[ptk])
                P.op("pe", lambda e, po=po, j=j, ptb=ptb, jl=jl: e.matmul(po[:, 0:512], lhsT=V[:, j, :], rhs=ptb, start=(j == 0), stop=(j == jl)), reads=["V", ptk], writes=[pok])
                P.op("pe", lambda e, pr=pr, j=j, ptb=ptb, jl=jl: e.matmul(pr[:, 0:512], lhsT=vones[:, j, :], rhs=ptb, start=(j == 0), stop=(j == jl)), reads=["vones", ptk], writes=[prk])
            P.op("dve", lambda e, pr=pr: e.reciprocal(out=rsb, in_=pr[:, 0:512]), reads=[prk], writes=["rsb"])
            oi = Q % 2
            ob, obk = oT[oi], "oT%d" % oi
            P.op("dve", lambda e, po=po, ob=ob: e.tensor_tensor(out=ob, in0=po[:, 0:512], in1=rsb, op=ALU.mult), reads=[pok, "rsb"], writes=[obk])
            P.dma("sp", C.attnT_s[128 * h:128 * h + 128, qc], ob, reads=[obk], writes=["attnT_s"])
    barrier(C)


def phase2a(C, stop=None):
    P, A = C.P, C.AR
    A.off = C.base_off
    QW = 256
    ynT = A.bf16(64 * QW).rearrange("p (k w) -> p k w", k=64)
    atT = A.bf16(64 * QW).rearrange("p (k w) -> p k w", k=64)
    hTq = A.bf16(32 * QW).rearrange("p (k w) -> p k w", k=32)
    wbs = [A.bf16(64 * 128).rearrange("p (k n) -> p k n", k=64) for _ in range(3)]
    wn = [0]
    bg = A.f32(64)
    gs = A.f32(QW); ga = A.f32(QW); t1 = A.f32(QW); t2 = A.f32(QW)
    mixT = A.bf16(32 * QW).rearrange("p (k w) -> p k w", k=32)
    P.dma("sp", bg, C.b_gate.rearrange("(m p) -> p m", p=128), writes=["bg"], allow_slow_non_contiguous=True)

    def loadw(src, c0, nk):
        i = wn[0]; wn[0] = (i + 1) % 3
        key = "wbs%d" % i
        P.dma("pool", wbs[i][:, 0:nk, :], src[:, c0:c0 + 128].rearrange("(k p) n -> p k n", p=128), writes=[key])
        return wbs[i], key
    nst = NQ // QW if stop != "p2a1" else 1
    for st in range(nst):
        qc = slice(QW * st, QW * st + QW)
        P.dma("sp", ynT, C.ynT_s.rearrange("(k p) t -> p k t", p=128)[:, :, qc], reads=["ynT_s"], writes=["ynT"])
        P.dma("sp", atT, C.attnT_s.rearrange("(k p) t -> p k t", p=128)[:, :, qc], reads=["attnT_s"], writes=["atT"])
        P.dma("sp", hTq, C.hT_s.rearrange("(k p) t -> p k t", p=128)[:, :, qc], reads=["hT_s"], writes=["hTq"])
        for m in range(32):
            outs = []
            for (src, c0, nk, act, akey) in ((C.w_ssm_proj, 128 * m, 64, ynT, "ynT"), (C.w_attn_proj, 128 * m, 64, atT, "atT"),
                                             (C.w_in, OG + 128 * m, 32, hTq, "hTq"), (C.w_in, OG + 4096 + 128 * m, 32, hTq, "hTq")):
                wbuf, wkey = loadw(src, c0, nk)
                pbk, pkey = bank(C, (0, 1, 2, 3, 4, 5))
                for kc in range(nk):
                    P.op("pe", lambda e, pbk=pbk, wbuf=wbuf, kc=kc, act=act, nk=nk: e.matmul(pbk[:, 0:QW], lhsT=wbuf[:, kc, :], rhs=act[:, kc, :], start=(kc == 0), stop=(kc == nk - 1)),
                         reads=[wkey, akey], writes=[pkey])
                outs.append((pbk, pkey))
            (p1, k1), (p2, k2), (p3, k3), (p4, k4) = outs
            P.op("act", lambda e, p3=p3, m=m: e.activation(out=gs, in_=p3[:, 0:QW], func=AF.Sigmoid, bias=bg[:, m:m + 1]), reads=[k3, "bg"], writes=["gs"])
            P.op("act", lambda e, p4=p4, m=m: e.activation(out=ga, in_=p4[:, 0:QW], func=AF.Sigmoid, bias=bg[:, 32 + m:33 + m]), reads=[k4, "bg"], writes=["ga"])
            P.op("dve", lambda e, p1=p1: e.tensor_tensor(out=t1, in0=p1[:, 0:QW], in1=gs, op=ALU.mult), reads=[k1, "gs"], writes=["t1"])
            P.op("dve", lambda e, p2=p2: e.tensor_tensor(out=t2, in0=p2[:, 0:QW], in1=ga, op=ALU.mult), reads=[k2, "ga"], writes=["t2"])
            P.op("pool", lambda e, m=m: e.tensor_tensor(out=mixT[:, m, :], in0=t1, in1=t2, op=ALU.add), reads=["t1", "t2"], writes=["mixT"])
        P.dma("sp", C.mixT_s.rearrange("(k p) t -> p k t", p=128)[:, :, qc], mixT, reads=["mixT"], writes=["mixT_s"])
    barrier(C)
    if stop == "p2a1":
        return

    A.off = C.base_off
    gin = A.f32(D); bin_ = A.f32(D); g1 = A.f32(D); b1 = A.f32(D)
    xt = A.f32(D); r = A.f32(D)
    mixTt = A.bf16(32 * 128).rearrange("p (k w) -> p k w", k=32)
    wo = [A.bf16(32 * 128).rearrange("p (k n) -> p k n", k=32) for _ in range(3)]
    won = [0]
    h1b = A.bf16(D)
    h1Tt = A.bf16(32 * 128).rearrange("p (k w) -> p k w", k=32)
    tr32 = [A.f32(128) for _ in range(2)]
    wr = A.f32(32 * 64).rearrange("p (k n) -> p k n", k=32)
    rb_bc = A.f32(64)
    identf = A.f32(128)
    stats = A.f32(48); mv = A.f32(2); rstd = A.f32(1); nmr = A.f32(1)
    sc_ = A.f32(64); ch_ = A.f32(64); eq_ = A.f32(64); msk = A.f32(64); cmk = A.f32(64); wsel = A.f32(64)
    m1 = A.f32(8); m2 = A.f32(8); gsc = A.f32(8); top8 = A.f32(8); gmask = A.f32(8); ssum = A.f32(1); rinv = A.f32(1)
    P.dma("sp", gin, C.ln_in_g.partition_broadcast(128), writes=["gin"])
    P.dma("sp", bin_, C.ln_in_b.partition_broadcast(128), writes=["bin"])
    P.dma("sp", g1, C.ln1_g.partition_broadcast(128), writes=["g1"])
    P.dma("sp", b1, C.ln1_b.partition_broadcast(128), writes=["b1"])
    P.dma("sp", wr, C.w_router.rearrange("(k p) n -> p k n", p=128), writes=["wr"])
    P.dma("sp", rb_bc, C.router_bias.partition_broadcast(128), writes=["rb_bc"])
    P.op("dve", lambda e: e.tensor_single_scalar(out=identf, in_=C.itf[:, 0:128], scalar=0.0, op=ALU.is_equal), reads=["itf"], writes=["identf"])
    v8 = lambda ap: ap.rearrange("p (g e) -> p g e", g=8)

    def ln_rows(src, gk, bk, gt, bt):
        key = src[1]; t_ = src[0]
        for c in range(8):
            P.op("dve", lambda e, c=c: e.bn_stats(out=stats[:, 6 * c:6 * c + 6], in_=t_[:, 512 * c:512 * c + 512]), reads=[key], writes=["stats%d" % c])
        P.op("dve", lambda e: e.bn_aggr(out=mv, in_=stats), reads=["stats%d" % c for c in range(8)], writes=["mv"])
        P.op("act", lambda e: e.activation(out=rstd, in_=mv[:, 1:2], func=AF.Sqrt, bias=C.epsln, scale=1.0), reads=["mv", "epsln"], writes=["rstd"])
        P.op("dve", lambda e: e.reciprocal(out=rstd, in_=rstd), reads=["rstd"], writes=["rstd"])
        P.op("dve", lambda e: e.scalar_tensor_tensor(out=nmr, in0=mv[:, 0:1], scalar=-1.0, in1=rstd, op0=ALU.mult, op1=ALU.mult), reads=["mv", "rstd"], writes=["nmr"])
        P.op("act", lambda e: e.activation(out=t_, in_=t_, func=AF.Identity, scale=rstd, bias=nmr), reads=[key, "rstd", "nmr"], writes=[key])
        P.op("dve", lambda e: e.tensor_tensor(out=t_, in0=t_, in1=gt, op=ALU.mult), reads=[key, gk], writes=[key])
        P.op("pool", lambda e: e.tensor_tensor(out=t_, in0=t_, in1=bt, op=ALU.add), reads=[key, bk], writes=[key])
    ntl = NQ // 128 if stop != "p2a2" else 1
    for tl in range(ntl):
        rows = slice(128 * tl, 128 * tl + 128)
        P.dma("sp", xt, C.xin[(QT0 + tl) * 128:(QT0 + tl) * 128 + 128, :], writes=["xt"])
        ln_rows((xt, "xt"), "gin", "bin", gin, bin_)
        P.dma("sp", mixTt, C.mixT_s.rearrange("(k p) t -> p k t", p=128)[:, :, rows], reads=["mixT_s"], writes=["mixTt"])
        for c4 in range(8):
            pbk, pkey = bank(C, (0, 1, 2, 3))
            for cb in range(4):
                col0 = 512 * c4 + 128 * cb
                i = won[0]; won[0] = (i + 1) % 3
                wkey = "wo%d" % i
                P.dma("pool", wo[i], C.w_out[:, col0:col0 + 128].rearrange("(k p) n -> p k n", p=128), writes=[wkey])
                for kc in range(32):
                    P.op("pe", lambda e, pbk=pbk, i=i, kc=kc, cb=cb: e.matmul(pbk[:, 128 * cb:128 * cb + 128], lhsT=mixTt[:, kc, :], rhs=wo[i][:, kc, :], start=(kc == 0), stop=(kc == 31)),
                         reads=[wkey, "mixTt"], writes=[pkey])
            cs = slice(512 * c4, 512 * c4 + 512)
            P.op("dve", lambda e, pbk=pbk, cs=cs: e.scalar_tensor_tensor(out=r[:, cs], in0=xt[:, cs], scalar=DN_ALPHA, in1=pbk[:, 0:512], op0=ALU.mult, op1=ALU.add), reads=["xt", pkey], writes=["r"])
        ln_rows((r, "r"), "g1", "b1", g1, b1)
        P.dma("sp", C.h1_s[rows, :], r, reads=["r"], writes=["h1_s"])
        P.op("act", lambda e: e.activation(out=h1b, in_=r, func=AF.Identity), reads=["r"], writes=["h1b"])
        for k8 in range(4):
            pt, pk = tbank(C)
            for j in range(8):
                kc = 8 * k8 + j
                P.op("pe", lambda e, pt=pt, j=j, kc=kc: e.transpose(out=pt[:, 128 * j:128 * j + 128], in_=h1b[:, 128 * kc:128 * kc + 128], identity=C.identb), reads=["h1b", "identb"], writes=[pk])
            P.op("act", lambda e, pt=pt, k8=k8: e.activation(out=h1Tt[:, 8 * k8:8 * k8 + 8, :], in_=pt[:, 0:1024].rearrange("p (k w) -> p k w", k=8), func=AF.Identity), reads=[pk], writes=["h1Tt"])
        P.dma("sp", C.h1T_s.rearrange("(k p) t -> p k t", p=128)[:, :, rows], h1Tt, reads=["h1Tt"], writes=["h1T_s"])
        plog, plk = C.pb[5], "pb5"
        for kc in range(32):
            ptf, ptk = bank(C, (6, 7))
            P.op("pe", lambda e, ptf=ptf, kc=kc: e.transpose(out=ptf[:, 0:128], in_=r[:, 128 * kc:128 * kc + 128], identity=identf), reads=["r", "identf"], writes=[ptk])
            ti_ = kc % 2
            P.op("dve" if kc % 2 else "act", (lambda e, ptf=ptf, ti_=ti_: e.tensor_copy(out=tr32[ti_], in_=ptf[:, 0:128])) if kc % 2 else
                 (lambda e, ptf=ptf, ti_=ti_: e.activation(out=tr32[ti_], in_=ptf[:, 0:128], func=AF.Identity)), reads=[ptk], writes=["tr32_%d" % ti_])
            P.op("pe", lambda e, kc=kc, ti_=ti_: e.matmul(plog[:, 0:64], lhsT=tr32[ti_], rhs=wr[:, kc, :], start=(kc == 0), stop=(kc == 31)), reads=["tr32_%d" % ti_, "wr"], writes=[plk])
        P.op("act", lambda e: e.activation(out=sc_, in_=plog[:, 0:64], func=AF.Sigmoid), reads=[plk], writes=["sc_"])
        P.op("dve", lambda e: e.tensor_tensor(out=ch_, in0=sc_, in1=rb_bc, op=ALU.add), reads=["sc_", "rb_bc"], writes=["ch_"])
        P.op("dve", lambda e: e.tensor_reduce(out=m1, in_=v8(ch_), axis=mybir.AxisListType.X, op=ALU.max), reads=["ch_"], writes=["m1"])
        P.op("dve", lambda e: e.tensor_tensor(out=v8(eq_), in0=v8(ch_), in1=m1.unsqueeze(2).broadcast_to([128, 8, 8]), op=ALU.is_equal), reads=["ch_", "m1"], writes=["eq_"])
        P.op("dve", lambda e: e.scalar_tensor_tensor(out=eq_, in0=eq_, scalar=-1e9, in1=ch_, op0=ALU.mult, op1=ALU.add), reads=["eq_", "ch_"], writes=["eq_"])
        P.op("dve", lambda e: e.tensor_reduce(out=m2, in_=v8(eq_), axis=mybir.AxisListType.X, op=ALU.max), reads=["eq_"], writes=["m2"])
        P.op("dve", lambda e: e.tensor_tensor(out=gsc, in0=m1, in1=m2, op=ALU.add), reads=["m1", "m2"], writes=["gsc"])
        P.op("dve", lambda e: e.max(out=top8, in_=gsc), reads=["gsc"], writes=["top8"])
        P.op("dve", lambda e: e.tensor_single_scalar(out=gmask, in_=gsc, scalar=top8[:, 3:4], op=ALU.is_ge), reads=["gsc", "top8"], writes=["gmask"])
        P.op("dve", lambda e: e.tensor_tensor(out=v8(msk), in0=v8(ch_), in1=gmask.unsqueeze(2).broadcast_to([128, 8, 8]), op=ALU.mult), reads=["ch_", "gmask"], writes=["msk"])
        P.op("dve", lambda e: e.tensor_scalar(out=gmask, in0=gmask, scalar1=-1.0, scalar2=1e9, op0=ALU.add, op1=ALU.mult), reads=["gmask", "msk"], writes=["gmask"])
        P.op("dve", lambda e: e.tensor_tensor(out=v8(cmk), in0=v8(msk), in1=gmask.unsqueeze(2).broadcast_to([128, 8, 8]), op=ALU.add), reads=["msk", "gmask"], writes=["cmk"])
        P.op("dve", lambda e: e.max(out=top8, in_=cmk), reads=["cmk", "gmask"], writes=["top8"])
        P.op("dve", lambda e: e.tensor_single_scalar(out=wsel, in_=cmk, scalar=top8[:, 7:8], op=ALU.is_ge), reads=["cmk", "top8"], writes=["wsel"])
        P.op("dve", lambda e: e.tensor_tensor(out=wsel, in0=wsel, in1=sc_, op=ALU.mult), reads=["wsel", "sc_"], writes=["wsel"])
        P.op("dve", lambda e: e.tensor_reduce(out=ssum, in_=wsel, axis=mybir.AxisListType.X, op=ALU.add), reads=["wsel"], writes=["ssum"])
        P.op("dve", lambda e: e.reciprocal(out=rinv, in_=ssum), reads=["ssum"], writes=["rinv"])
        P.op("dve", lambda e: e.tensor_scalar(out=wsel, in0=wsel, scalar1=rinv, scalar2=2.5, op0=ALU.mult, op1=ALU.mult), reads=["wsel", "rinv"], writes=["wsel"])
        P.dma("sp", C.wts_s[rows, :], wsel, reads=["wsel"], writes=["wts_s"])
    barrier(C)


def moe(C, yout, stop=None):
    P, A = C.P, C.AR
    A.off = C.base_off
    GW = 512
    hT1 = A.bf16(32 * GW).rearrange("p (k w) -> p k w", k=32)
    acc = A.f32(4 * D).rearrange("p (t c) -> p t c", t=4)
    wts = A.f32(4 * 64).rearrange("p (t e) -> p t e", t=4)
    Hs = A.bf16(6 * GW).rearrange("p (j w) -> p j w", j=6)
    ovl = A.off
    wblk = [A.bf16(8 * 768).rearrange("p (k f) -> p k f", k=8) for _ in range(2)]
    wdh = [A.bf16(6 * 2048).rearrange("p (j n) -> p j n", j=6) for _ in range(2)]
    endw = A.off
    A.off = ovl
    h1t = A.f32(D); g2 = A.f32(D); b2 = A.f32(D)
    stats = A.f32(48); mv = A.f32(2); rstd = A.f32(1); nmr = A.f32(1)
    A.off = max(A.off, endw)
    wbn = [0]; wdn = [0]
    ngr = NQ // GW if stop != "moe1" else 1
    experts = [("sh", C.w_sh_gate, C.w_sh_up, C.w_sh_down)] + [(e, C.w_exp_gate[e], C.w_exp_up[e], C.w_exp_down[e]) for e in range(64)]
    if stop == "moe1":
        experts = experts[:3]
    for G in range(ngr):
        gc = slice(GW * G, GW * G + GW)
        P.dma("sp", hT1, C.h1T_s.rearrange("(k p) t -> p k t", p=128)[:, :, gc], reads=["h1T_s"], writes=["hT1"])
        P.dma("sp", wts, C.wts_s[gc, :].rearrange("(t p) e -> p t e", p=128), reads=["wts_s"], writes=["wts"])
        for xi, (eid, wg_, wu_, wd_) in enumerate(experts):
            for which, wsrc in (("g", wg_), ("u", wu_)):
                for kg in range(4):
                    i = wbn[0]; wbn[0] = 1 - i
                    wkey = "wblk%d" % i
                    P.dma("pool", wblk[i], wsrc[1024 * kg:1024 * kg + 1024, :].rearrange("(k p) f -> p k f", p=128), writes=[wkey])
                    for j in range(6):
                        for kl in range(8):
                            P.op("pe", lambda e, j=j, kl=kl, kg=kg, i=i: e.matmul(C.pb[j][:, 0:GW], lhsT=wblk[i][:, kl, 128 * j:128 * j + 128], rhs=hT1[:, 8 * kg + kl, :],
                                                                           start=(kg == 0 and kl == 0), stop=(kg == 3 and kl == 7)), reads=[wkey, "hT1"], writes=["pb%d" % j])
                for j in range(6):
                    if which == "g":
                        P.op("act", lambda e, j=j: e.activation(out=Hs[:, j, :], in_=C.pb[j][:, 0:GW], func=AF.Silu), reads=["pb%d" % j], writes=["Hs%d" % j])
                    else:
                        P.op("dve", lambda e, j=j: e.tensor_tensor(out=Hs[:, j, :], in0=Hs[:, j, :], in1=C.pb[j][:, 0:GW], op=ALU.mult), reads=["Hs%d" % j, "pb%d" % j], writes=["Hs%d" % j])
            for c2 in range(2):
                i = wdn[0]; wdn[0] = 1 - i
                dkey = "wdh%d" % i
                P.dma("pool", wdh[i], wd_[:, 2048 * c2:2048 * c2 + 2048].rearrange("(j p) n -> p j n", p=128), writes=[dkey])
                for tt in range(4):
                    for n4 in range(4):
                        pbk, pkey = bank(C, (6, 7))
                        for j in range(6):
                            P.op("pe", lambda e, pbk=pbk, j=j, tt=tt, n4=n4, i=i: e.matmul(pbk[:, 0:512], lhsT=Hs[:, j, 128 * tt:128 * tt + 128], rhs=wdh[i][:, j, 512 * n4:512 * n4 + 512],
                                                                                 start=(j == 0), stop=(j == 5)), reads=[dkey] + ["Hs%d" % j for j in range(6)], writes=[pkey])
                        cs = slice(2048 * c2 + 512 * n4, 2048 * c2 + 512 * n4 + 512)
                        akey = "acc%d_%d" % (tt, c2 * 4 + n4)
                        if xi == 0:
                            P.op("act", lambda e, pbk=pbk, tt=tt, cs=cs: e.activation(out=acc[:, tt, cs], in_=pbk[:, 0:512], func=AF.Identity), reads=[pkey], writes=[akey])
                        else:
                            P.op("dve", lambda e, pbk=pbk, tt=tt, cs=cs, eid=eid: e.scalar_tensor_tensor(out=acc[:, tt, cs], in0=pbk[:, 0:512], scalar=wts[:, tt, eid:eid + 1], in1=acc[:, tt, cs],
                                                                                            op0=ALU.mult, op1=ALU.add), reads=[pkey, "wts", akey], writes=[akey])
        barrier(C)
        P.dma("sp", g2, C.ln2_g.partition_broadcast(128), writes=["g2"])
        P.dma("sp", b2, C.ln2_b.partition_broadcast(128), writes=["b2"])
        for tt in range(4):
            rows = slice(GW * G + 128 * tt, GW * G + 128 * tt + 128)
            P.dma("sp", h1t, C.h1_s[rows, :], reads=["h1_s"], writes=["h1t"])
            a_ = acc[:, tt, :]
            ak = "accf%d" % tt
            P.op("dve", lambda e, a_=a_: e.scalar_tensor_tensor(out=a_, in0=h1t, scalar=DN_ALPHA, in1=a_, op0=ALU.mult, op1=ALU.add), reads=["h1t"], writes=[ak])
            for c in range(8):
                P.op("dve", lambda e, c=c, a_=a_: e.bn_stats(out=stats[:, 6 * c:6 * c + 6], in_=a_[:, 512 * c:512 * c + 512]), reads=[ak], writes=["stats%d" % c])
            P.op("dve", lambda e: e.bn_aggr(out=mv, in_=stats), reads=["stats%d" % c for c in range(8)], writes=["mv"])
            P.op("act", lambda e: e.activation(out=rstd, in_=mv[:, 1:2], func=AF.Sqrt, bias=C.epsln, scale=1.0), reads=["mv", "epsln"], writes=["rstd"])
            P.op("dve", lambda e: e.reciprocal(out=rstd, in_=rstd), reads=["rstd"], writes=["rstd"])
            P.op("dve", lambda e: e.scalar_tensor_tensor(out=nmr, in0=mv[:, 0:1], scalar=-1.0, in1=rstd, op0=ALU.mult, op1=ALU.mult), reads=["mv", "rstd"], writes=["nmr"])
            P.op("act", lambda e, a_=a_: e.activation(out=a_, in_=a_, func=AF.Identity, scale=rstd, bias=nmr), reads=[ak, "rstd", "nmr"], writes=[ak])
            P.op("dve", lambda e, a_=a_: e.tensor_tensor(out=a_, in0=a_, in1=g2, op=ALU.mult), reads=[ak, "g2"], writes=[ak])
            P.op("pool", lambda e, a_=a_: e.tensor_tensor(out=a_, in0=a_, in1=b2, op=ALU.add), reads=[ak, "b2"], writes=[ak])
            P.dma("sp", yout[rows, :], a_, reads=[ak], writes=["yout"], is_out=True)
        barrier(C)


WNAMES = ["ln_in_g", "ln_in_b", "w_in", "b_gate", "conv_w", "conv_b", "dt_bias", "a_log", "d_skip", "ssm_norm_g", "w_ssm_proj", "q_a_norm_g", "w_q_b",
          "kv_a_norm_g", "w_kv_b", "w_attn_proj", "w_out", "ln1_g", "ln1_b", "w_router", "router_bias", "w_exp_gate", "w_exp_up", "w_exp_down",
          "w_sh_gate", "w_sh_up", "w_sh_down", "ln2_g", "ln2_b"]
LNAMES = ["xin", "validtm", "validrow", "posraw", "posoff"]


def build_program():
    nc = bass.Bass("TRN2", target_bir_lowering=False)
    C = setup(nc, LNAMES + WNAMES)
    yout = nc.dram_tensor("yout", [NQ, D], F32, kind="ExternalOutput").ap()
    consts(C)
    barrier(C)
    phase1(C)
    attention(C)
    phase2a(C)
    moe(C, yout)
    C.P.finish()
    C.P.emit()
    return nc


def _core_inputs(c, x, positions, meta_tokens):
    b, j = c // JPB, c % JPB
    fp = (JPB - 1 - j) * (NQ // 128) * 128
    m0 = fp + 112
    n = NQ * (j + 1)
    xin = np.zeros((L, D), np.float32)
    xin[m0:m0 + 16] = meta_tokens
    xin[m0 + 16:] = x[b, :n]
    valid = np.zeros(L, np.float32)
    valid[m0:] = 1.0
    posraw = np.zeros((1, L), np.int32)
    posraw[0, m0 + 16:] = positions[b, :n]
    posoff = np.zeros((1, L), np.int32)
    posoff[0, m0:m0 + 16] = np.arange(16, dtype=np.int32)
    posoff[0, m0 + 16:] = 16
    return dict(xin=xin, validtm=np.ascontiguousarray(valid.reshape(NT, 128).T), validrow=valid.reshape(1, L), posraw=posraw, posoff=posoff)


def kernel(**inputs):
    x = np.asarray(inputs["x"], np.float32)
    positions = np.asarray(inputs["positions"]).astype(np.int32)
    meta = np.asarray(inputs["meta_tokens"], np.float32)
    shared = {}
    for n in WNAMES:
        a = np.asarray(inputs[n])
        if n not in ("ln_in_g", "ln_in_b"):
            a = a[0]
        shared[n] = np.ascontiguousarray(a, dtype=np.float32)
    nc = build_program()
    in_maps = []
    for c in range(NCORES):
        m = dict(shared)
        m.update(_core_inputs(c, x, positions, meta))
        in_maps.append(m)
    res = run_bass_kernel_spmd(nc, in_maps, core_ids=list(range(NCORES)))
    out = np.zeros((2, 4096, D), np.float32)
    for c in range(NCORES):
        b, j = c // JPB, c % JPB
        out[b, NQ * j:NQ * (j + 1)] = np.asarray(res.results[c]["yout"], np.float32)
    return out
```
